# Optimizing a Trainium2 kernel written in Bass

```python
import math
import jax, jax.numpy as jnp
from jax import lax
import numpy as np

D_MODEL = 1024
BATCH = 4
SEQ = 8192
DEPTH = 2

GRID_W = 64
CTX_LEN = 256
EPS = 1e-6
ROPE_BASE = 10000.0
NEG_INF = -1e30
BLOCK_Q = 128

DA_HEADS = 4
DA_DIM = 64
RET_HEADS = 4
RET_QK = 64
RET_V = 128
RET_CHUNK = 128
DA_QK_W = DA_HEADS * 2 * DA_DIM
DA_V_W = DA_HEADS * 2 * DA_DIM
RET_QK_W = RET_HEADS * RET_QK
RET_V_W = RET_HEADS * RET_V
EVEN_WIDTHS = (DA_QK_W, DA_QK_W, DA_V_W, RET_QK_W, RET_QK_W, RET_V_W, RET_V_W)
EVEN_IN = DA_QK_W * 2 + DA_V_W + RET_QK_W * 2 + RET_V_W * 2
EVEN_OUT = DA_V_W + RET_V_W

SW_Q_HEADS = 16
SW_KV_HEADS = 2
SW_DIM = 64
WINDOW = 128
ODD_IN = (SW_Q_HEADS + 2 * SW_KV_HEADS) * SW_DIM
ODD_OUT = SW_Q_HEADS * SW_DIM

N_EXPERTS = 32
TOP_K = 4
D_FF = 1024
SWIGLU_ALPHA = 1.702
SWIGLU_LIMIT = 7.0
MOE_BLOCK = 128

kernel_name = 'hybrid_diffattn_retention_swa_moe_dit'


def rmsnorm(x, w):
    xf = x.astype(jnp.float32)
    y = xf * lax.rsqrt(jnp.mean(xf * xf, axis=-1, keepdims=True) + EPS)
    return (y * w.astype(jnp.float32)).astype(x.dtype)


def adaln(cvec, w, b):
    m = (jax.nn.silu(cvec) @ w + b)[..., None, :]
    return jnp.split(m, 6, axis=-1)


def axial_rope(x, row, col):
    hd = x.shape[-1]
    p = hd // 2
    f = p // 2
    inv = ROPE_BASE ** (-jnp.arange(f, dtype=jnp.float32) / f)
    xf = x.astype(jnp.float32)

    def rot(part, pos):
        ang = pos[:, None] * inv[None, :]
        cos = jnp.cos(ang)[:, None, :]
        sin = jnp.sin(ang)[:, None, :]
        a, b = part[..., :f], part[..., f:]
        return jnp.concatenate([a * cos - b * sin, a * sin + b * cos], axis=-1)

    out = jnp.concatenate([rot(xf[..., :p], row), rot(xf[..., p:], col)], axis=-1)
    return out.astype(x.dtype)


def split_heads(t, n_heads):
    B, N, _ = t.shape
    return t.reshape(B, N, n_heads, -1).transpose(0, 2, 1, 3)


def diff_attention(lat, ctx, lam, need_ctx):
    q, k, v = lat
    qc, kc, vc = ctx
    B, H, _, S, d = q.shape
    scale = d ** -0.5
    k_all = jnp.concatenate([kc, k], axis=3)
    v_all = jnp.concatenate([vc, v], axis=2)

    def attend(qb, keys, vals):
        s = jnp.einsum('bhcqd,bhckd->bhcqk', qb, keys).astype(jnp.float32) * scale
        p = jax.nn.softmax(s, axis=-1)
        a = p[:, :, 0] - lam * p[:, :, 1]
        return jnp.einsum('bhqk,bhke->bhqe', a.astype(vals.dtype), vals)

    nb = S // BLOCK_Q
    qb = jnp.moveaxis(q.reshape(B, H, 2, nb, BLOCK_Q, d), 3, 0)
    o = lax.map(lambda blk: attend(blk, k_all, v_all), qb)
    o_lat = jnp.moveaxis(o, 0, 2).reshape(B, H, S, 2 * d)
    o_ctx = attend(qc, kc, vc) if need_ctx else None
    return o_lat, o_ctx


def retention_chunkwise(q, k, v, log_gamma, state0):
    B, H, N, dk = q.shape
    dv = v.shape[-1]
    C = RET_CHUNK
    nc = N // C
    qc = q.reshape(B, H, nc, C, dk).astype(jnp.float32)
    kc = k.reshape(B, H, nc, C, dk).astype(jnp.float32)
    vc = v.reshape(B, H, nc, C, dv).astype(jnp.float32)
    i = jnp.arange(C, dtype=jnp.float32)
    lg = log_gamma[:, None, None]
    diff = i[:, None] - i[None, :]
    decay_in = jnp.where(diff >= 0, jnp.exp(lg * jnp.maximum(diff, 0.0)), 0.0)
    scores = jnp.einsum('bhnid,bhnjd->bhnij', qc, kc) * decay_in[:, None]
    inner = jnp.einsum('bhnij,bhnje->bhnie', scores, vc)
    k_dec = kc * jnp.exp(lg * (C - 1 - i))[..., None]
    chunk_kv = jnp.einsum('bhnjd,bhnje->nbhde', k_dec, vc)
    gamma_c = jnp.exp(log_gamma * C)[None, :, None, None]

    def step(state, kv):
        return gamma_c * state + kv, state

    final, prev_states = lax.scan(step, state0, chunk_kv)
    q_dec = qc * jnp.exp(lg * (i + 1))[..., None]
    cross = jnp.einsum('bhnid,nbhde->bhnie', q_dec, prev_states)
    return (inner + cross).reshape(B, H, N, dv), final


def bidir_retention(lat, ctx, decay_logit, need_ctx):
    q, k, v = lat
    qc, kc, vc = ctx
    B, H, _, dk = q.shape
    dv = v.shape[-1]
    log_gamma = jax.nn.log_sigmoid(decay_logit.astype(jnp.float32))
    zero = jnp.zeros((B, H, dk, dv), jnp.float32)
    flip = lambda t: t[:, :, ::-1]
    o_cf, s_f = retention_chunkwise(qc, kc, vc, log_gamma[0], zero)
    o_cb, s_b = retention_chunkwise(flip(qc), flip(kc), flip(vc), log_gamma[1], zero)
    o_lf, _ = retention_chunkwise(q, k, v, log_gamma[0], s_f)
    o_lb, _ = retention_chunkwise(flip(q), flip(k), flip(v), log_gamma[1], s_b)
    o_lat = (o_lf + flip(o_lb)).astype(v.dtype)
    o_ctx = (o_cf + flip(o_cb)).astype(v.dtype) if need_ctx else None
    return o_lat, o_ctx


def even_mixer(a_lat, a_ctx, w_in, w_out, lam_p, dnorm_w, decay_logit, rnorm_w, lam_init, row, col, need_ctx):
    cuts = np.cumsum(EVEN_WIDTHS)[:-1].tolist()

    def project(a, rope):
        B, N, _ = a.shape
        dq, dk, dv, rq, rk, rv, rg = jnp.split(a @ w_in, cuts, axis=-1)
        dq = dq.reshape(B, N, 2 * DA_HEADS, DA_DIM)
        dk = dk.reshape(B, N, 2 * DA_HEADS, DA_DIM)
        if rope:
            dq = axial_rope(dq, row, col)
            dk = axial_rope(dk, row, col)
        dq = dq.reshape(B, N, DA_HEADS, 2, DA_DIM).transpose(0, 2, 3, 1, 4)
        dk = dk.reshape(B, N, DA_HEADS, 2, DA_DIM).transpose(0, 2, 3, 1, 4)
        dv = split_heads(dv, DA_HEADS)
        rq = split_heads(rq, RET_HEADS) * (RET_QK ** -0.5)
        rk = split_heads(rk, RET_HEADS)
        rv = split_heads(rv, RET_HEADS)
        return (dq, dk, dv), (rq, rk, rv), rg

    da_lat, rt_lat, g_lat = project(a_lat, True)
    da_ctx, rt_ctx, g_ctx = project(a_ctx, False)
    lam = (jnp.exp(jnp.sum(lam_p[0] * lam_p[1])) - jnp.exp(jnp.sum(lam_p[2] * lam_p[3]))).astype(jnp.float32) + lam_init
    d_lat, d_ctx = diff_attention(da_lat, da_ctx, lam, need_ctx)
    r_lat, r_ctx = bidir_retention(rt_lat, rt_ctx, decay_logit, need_ctx)

    def finish(d, r, g):
        B, H, N, _ = d.shape
        d = rmsnorm(d, dnorm_w) * (1.0 - lam_init)
        r = rmsnorm(r, rnorm_w)
        d = d.transpose(0, 2, 1, 3).reshape(B, N, -1)
        r = r.transpose(0, 2, 1, 3).reshape(B, N, -1) * jax.nn.silu(g)
        return jnp.concatenate([d, r], axis=-1) @ w_out

    out_ctx = finish(d_ctx, r_ctx, g_ctx) if need_ctx else None
    return finish(d_lat, r_lat, g_lat), out_ctx


def window_attention(lat, ctx, sinks, need_ctx):
    q, k, v = lat
    qc, kc, vc = ctx
    B, Hkv, G, S, dh = q.shape
    scale = dh ** -0.5
    span = BLOCK_Q + 2 * WINDOW
    nb = S // BLOCK_Q
    pad = ((0, 0), (0, 0), (WINDOW, WINDOW), (0, 0))
    k_pad = jnp.pad(k, pad)
    v_pad = jnp.pad(v, pad)
    sink = sinks.astype(jnp.float32)[None, :, :, None, None]

    def softmax_with_sink(logits):
        full = jnp.concatenate([logits, jnp.broadcast_to(sink, logits.shape[:-1] + (1,))], axis=-1)
        return jax.nn.softmax(full, axis=-1)[..., :-1]

    def block(args):
        qb, n = args
        start = n * BLOCK_Q
        kb = lax.dynamic_slice_in_dim(k_pad, start, span, axis=2)
        vb = lax.dynamic_slice_in_dim(v_pad, start, span, axis=2)
        qpos = start + jnp.arange(BLOCK_Q)
        kpos = start - WINDOW + jnp.arange(span)
        valid = (jnp.abs(qpos[:, None] - kpos[None, :]) <= WINDOW) & (kpos >= 0) & (kpos < S)
        s_band = jnp.einsum('bhgqd,bhkd->bhgqk', qb, kb).astype(jnp.float32) * scale
        s_band = jnp.where(valid, s_band, NEG_INF)
        s_ctx = jnp.einsum('bhgqd,bhkd->bhgqk', qb, kc).astype(jnp.float32) * scale
        p = softmax_with_sink(jnp.concatenate([s_band, s_ctx], axis=-1)).astype(v.dtype)
        return (jnp.einsum('bhgqk,bhkd->bhgqd', p[..., :span], vb)
                + jnp.einsum('bhgqk,bhkd->bhgqd', p[..., span:], vc))

    qb = jnp.moveaxis(q.reshape(B, Hkv, G, nb, BLOCK_Q, dh), 3, 0)
    o = lax.map(block, (qb, jnp.arange(nb)))
    o_lat = jnp.moveaxis(o, 0, 3).reshape(B, Hkv, G, S, dh)
    o_ctx = None
    if need_ctx:
        s = jnp.einsum('bhgqd,bhkd->bhgqk', qc, kc).astype(jnp.float32) * scale
        o_ctx = jnp.einsum('bhgqk,bhkd->bhgqd', softmax_with_sink(s).astype(vc.dtype), vc)
    return o_lat, o_ctx


def odd_mixer(a_lat, a_ctx, w_qkv, w_out, sinks, row, col, need_ctx):
    G = SW_Q_HEADS // SW_KV_HEADS
    cuts = [SW_Q_HEADS * SW_DIM, (SW_Q_HEADS + SW_KV_HEADS) * SW_DIM]

    def project(a, rope):
        B, N, _ = a.shape
        q, k, v = jnp.split(a @ w_qkv, cuts, axis=-1)
        q = q.reshape(B, N, SW_Q_HEADS, SW_DIM)
        k = k.reshape(B, N, SW_KV_HEADS, SW_DIM)
        if rope:
            q = axial_rope(q, row, col)
            k = axial_rope(k, row, col)
        q = q.reshape(B, N, SW_KV_HEADS, G, SW_DIM).transpose(0, 2, 3, 1, 4)
        k = k.transpose(0, 2, 1, 3)
        v = split_heads(v, SW_KV_HEADS)
        return q, k, v

    o_lat, o_ctx = window_attention(project(a_lat, True), project(a_ctx, False),
                                    sinks.reshape(SW_KV_HEADS, G), need_ctx)

    def finish(o):
        B, _, _, N, _ = o.shape
        return o.transpose(0, 3, 1, 2, 4).reshape(B, N, -1) @ w_out

    return finish(o_lat), (finish(o_ctx) if need_ctx else None)


def moe(x, router_w, router_b, w1, b1, w2, b2):
    T, D = x.shape
    logits = (x @ router_w).astype(jnp.float32) + router_b.astype(jnp.float32)
    top_v, top_i = lax.top_k(logits, TOP_K)
    gate = jax.nn.softmax(top_v, axis=-1)
    M = T * TOP_K
    e_flat = top_i.reshape(M)
    tok_flat = jnp.repeat(jnp.arange(T, dtype=jnp.int32), TOP_K)
    order = jnp.argsort(e_flat)
    e_sorted = e_flat[order]
    counts = jnp.bincount(e_flat, length=N_EXPERTS)
    starts = jnp.cumsum(counts) - counts
    padded = ((counts + MOE_BLOCK - 1) // MOE_BLOCK) * MOE_BLOCK
    pad_ends = jnp.cumsum(padded)
    pad_starts = pad_ends - padded
    dest = pad_starts[e_sorted] + (jnp.arange(M) - starts[e_sorted])
    n_blocks = -(-M // MOE_BLOCK) + N_EXPERTS
    P = n_blocks * MOE_BLOCK
    src = jnp.full((P,), T, jnp.int32).at[dest].set(tok_flat[order])
    row_gate = jnp.zeros((P,), jnp.float32).at[dest].set(gate.reshape(M)[order])
    x_buf = jnp.concatenate([x, jnp.zeros((1, D), x.dtype)], axis=0)[src]
    block_expert = jnp.minimum(jnp.searchsorted(pad_ends, jnp.arange(n_blocks) * MOE_BLOCK, side='right'),
                               N_EXPERTS - 1)

    def expert_block(args):
        xb, e = args
        h = xb @ w1[e] + b1[e]
        glu = jnp.minimum(h[:, :D_FF], SWIGLU_LIMIT)
        lin = jnp.clip(h[:, D_FF:], -SWIGLU_LIMIT, SWIGLU_LIMIT)
        act = glu * jax.nn.sigmoid(SWIGLU_ALPHA * glu) * (lin + 1.0)
        return act @ w2[e] + b2[e]

    y_buf = lax.map(expert_block, (x_buf.reshape(n_blocks, MOE_BLOCK, D), block_expert))
    y = y_buf.reshape(P, D) * row_gate[:, None].astype(y_buf.dtype)
    return jax.ops.segment_sum(y, src, num_segments=T + 1)[:T]


def setup_inputs(seed: int = 0) -> dict:
    key = jax.random.key(seed)
    ks = list(jax.random.split(key, 32))
    n_even = (DEPTH + 1) // 2
    n_odd = DEPTH // 2

    def nrm(idx, shape, scale):
        return jax.random.normal(ks[idx], shape, jnp.float32) * scale

    g0 = 1.0 - 2.0 ** (-5.0 - np.arange(RET_HEADS))
    base_logit = jnp.asarray(np.log(g0 / (1.0 - g0)), jnp.float32)
    return {
        'x': nrm(0, (BATCH, SEQ, D_MODEL), 1.0),
        'c': nrm(1, (BATCH, D_MODEL), 1.0),
        'ctx': nrm(2, (BATCH, CTX_LEN, D_MODEL), 1.0),
        'c_ctx': nrm(3, (D_MODEL,), 1.0),
        'mod_w': nrm(4, (DEPTH, D_MODEL, 6 * D_MODEL), 0.5 * D_MODEL ** -0.5),
        'mod_b': nrm(5, (DEPTH, 6 * D_MODEL), 0.02),
        'norm1_w': 1.0 + nrm(6, (DEPTH, D_MODEL), 0.02),
        'norm2_w': 1.0 + nrm(7, (DEPTH, D_MODEL), 0.02),
        'even_w_in': nrm(8, (n_even, D_MODEL, EVEN_IN), D_MODEL ** -0.5),
        'even_w_out': nrm(9, (n_even, EVEN_OUT, D_MODEL), EVEN_OUT ** -0.5),
        'diff_lam': nrm(10, (n_even, 4, DA_DIM), 0.1),
        'diff_norm_w': 1.0 + nrm(11, (n_even, 2 * DA_DIM), 0.02),
        'ret_decay_logit': base_logit[None, None, :] + nrm(12, (n_even, 2, RET_HEADS), 0.05),
        'ret_norm_w': 1.0 + nrm(13, (n_even, RET_V), 0.02),
        'odd_w_qkv': nrm(14, (n_odd, D_MODEL, ODD_IN), D_MODEL ** -0.5),
        'odd_w_out': nrm(15, (n_odd, ODD_OUT, D_MODEL), ODD_OUT ** -0.5),
        'odd_sinks': nrm(16, (n_odd, SW_Q_HEADS), 0.5),
        'router_w': nrm(17, (DEPTH, D_MODEL, N_EXPERTS), D_MODEL ** -0.5),
        'router_b': nrm(18, (DEPTH, N_EXPERTS), 0.01),
        'moe_w1': nrm(19, (DEPTH, N_EXPERTS, D_MODEL, 2 * D_FF), D_MODEL ** -0.5),
        'moe_b1': nrm(20, (DEPTH, N_EXPERTS, 2 * D_FF), 0.01),
        'moe_w2': nrm(21, (DEPTH, N_EXPERTS, D_FF, D_MODEL), D_FF ** -0.5),
        'moe_b2': nrm(22, (DEPTH, N_EXPERTS, D_MODEL), 0.01),
        'final_norm_w': 1.0 + nrm(23, (D_MODEL,), 0.02),
    }


def reference(x, c, ctx, c_ctx, mod_w, mod_b, norm1_w, norm2_w,
              even_w_in, even_w_out, diff_lam, diff_norm_w, ret_decay_logit, ret_norm_w,
              odd_w_qkv, odd_w_out, odd_sinks,
              router_w, router_b, moe_w1, moe_b1, moe_w2, moe_b2, final_norm_w):
    B, S, D = x.shape
    L = ctx.shape[1]
    n_rows = S // GRID_W
    t = jnp.arange(n_rows * GRID_W)
    row = (t // GRID_W).astype(jnp.float32)
    col = (t % GRID_W).astype(jnp.float32)

    h_lat, h_ctx = x, ctx
    for layer in range(DEPTH):
        last = layer == DEPTH - 1
        sh1, sc1, g1, sh2, sc2, g2 = adaln(c, mod_w[layer], mod_b[layer])
        csh1, csc1, cg1, csh2, csc2, cg2 = adaln(c_ctx, mod_w[layer], mod_b[layer])
        a_lat = rmsnorm(h_lat, norm1_w[layer]) * (1.0 + sc1) + sh1
        a_ctx = rmsnorm(h_ctx, norm1_w[layer]) * (1.0 + csc1) + csh1
        if layer % 2 == 0:
            i = layer // 2
            lam_init = 0.8 - 0.6 * math.exp(-0.3 * layer)
            m_lat, m_ctx = even_mixer(a_lat, a_ctx, even_w_in[i], even_w_out[i], diff_lam[i], diff_norm_w[i],
                                      ret_decay_logit[i], ret_norm_w[i], lam_init, row, col, not last)
        else:
            i = layer // 2
            m_lat, m_ctx = odd_mixer(a_lat, a_ctx, odd_w_qkv[i], odd_w_out[i], odd_sinks[i], row, col, not last)
        h_lat = h_lat + g1 * m_lat
        f_lat = rmsnorm(h_lat, norm2_w[layer]) * (1.0 + sc2) + sh2
        if last:
            y = moe(f_lat.reshape(B * S, D), router_w[layer], router_b[layer],
                    moe_w1[layer], moe_b1[layer], moe_w2[layer], moe_b2[layer])
            h_lat = h_lat + g2 * y.reshape(B, S, D)
        else:
            h_ctx = h_ctx + cg1 * m_ctx
            f_ctx = rmsnorm(h_ctx, norm2_w[layer]) * (1.0 + csc2) + csh2
            tokens = jnp.concatenate([f_lat.reshape(B * S, D), f_ctx.reshape(B * L, D)], axis=0)
            y = moe(tokens, router_w[layer], router_b[layer],
                    moe_w1[layer], moe_b1[layer], moe_w2[layer], moe_b2[layer])
            h_lat = h_lat + g2 * y[:B * S].reshape(B, S, D)
            h_ctx = h_ctx + cg2 * y[B * S:].reshape(B, L, D)
    return rmsnorm(h_lat, final_norm_w)
```

```python
import os
import numpy as np
from contextlib import ExitStack
import concourse.bass as bass
import concourse.mybir as mybir
from concourse.bass_utils import run_bass_kernel_spmd

F32 = mybir.dt.float32
BF16 = mybir.dt.bfloat16
AF = mybir.ActivationFunctionType
ALU = mybir.AluOpType
AX = mybir.AxisListType
ENG = ('pe', 'act', 'dve', 'pool', 'sp')

SEQ = 8192
CTXL = 256
NE = SEQ + CTXL
NT_ALL = NE // 128
NT_Q = 35
NQ = NT_Q * 128
NEXP = 32


class Sched:
    ROLL = 30000
    NDMA = 12

    def __init__(self, nc, stack, parent=None, setid=0):
        self.parent, self.setid = parent, setid
        if parent is not None:
            self.NDMA = 4
        self.nc, self.stack = nc, stack
        self.E = {'pe': nc.tensor, 'act': nc.scalar, 'dve': nc.vector, 'pool': nc.gpsimd, 'sp': nc.sync}
        self.cnt = {e: 0 for e in ENG}
        self.dcnt = {e: 0 for e in ENG}
        self.sems = {}
        self.waited = {e: {} for e in ENG}
        self.bufs = {}
        self.last = {}
        self.nwaits = 0

    def sem(self, key):
        if self.parent is not None:
            return self.parent.sem(('c', self.setid) + tuple(key))
        s = self.sems.get(key)
        if s is None:
            s = self.stack.enter_context(self.nc.semaphore("s_" + "_".join(map(str, key))))
            self.sems[key] = s
        return s

    def _wait(self, e, k, v):
        wd = self.waited[e]
        if wd.get(k, 0) >= v:
            return
        wd[k] = v
        self.E[e].wait_ge(self.sem(k), v)
        self.nwaits += 1

    def op(self, e, fn, reads=(), writes=(), dma=False):
        need = {}
        for r in reads:
            b = self.bufs.get(r)
            if b:
                for k, v in b[0].items():
                    if need.get(k, 0) < v:
                        need[k] = v
        for w in writes:
            b = self.bufs.get(w)
            if b:
                for d in b:
                    for k, v in d.items():
                        if need.get(k, 0) < v:
                            need[k] = v
        if dma:
            m = self.dcnt[e]
            self.dcnt[e] += 1
            sk = (e, 'd', m % self.NDMA)
            val = 16 * (m // self.NDMA + 1)
            if m >= self.NDMA:
                need[sk] = max(need.get(sk, 0), val - 16)
            inc = 16
        else:
            n = self.cnt[e]
            self.cnt[e] += 1
            sk = (e, 'c', n // self.ROLL)
            val = n % self.ROLL + 1
            inc = 1
        for k, v in need.items():
            if e == 'pe' and k[0] == 'pe' and k[1] == 'c':
                continue
            self._wait(e, k, v)
        fn(self.E[e]).then_inc(self.sem(sk), inc)
        self.last[sk] = val
        for r in reads:
            b = self.bufs.setdefault(r, [{}, {}])
            if b[1].get(sk, 0) < val:
                b[1][sk] = val
        for w in writes:
            self.bufs[w] = [{sk: val}, {}]

    def barrier(self):
        for e in ENG:
            for k, v in self.last.items():
                if k[0] == e and k[1] == 'c':
                    continue
                self._wait(e, k, v)
        self.bufs = {}

    def finish(self):
        for k, v in self.last.items():
            self._wait('sp', k, v)

    def sync_keys(self, keys):
        for key in keys:
            b = self.bufs.get(key)
            if b:
                for e in ENG:
                    for k, v in b[0].items():
                        if e == 'pe' and k[0] == 'pe' and k[1] == 'c':
                            continue
                        self._wait(e, k, v)

    def unit_begin(self):
        u = getattr(self, 'units', 0)
        if u > 0:
            hs = self.sem(('HS',))
            for e in ENG:
                self.E[e].wait_ge(hs, u)
        return Sched(self.nc, self.stack, parent=self, setid=u % 2)

    def unit_end(self, child):
        child.barrier()
        other = 1 - child.setid
        sp = self.E['sp']
        for key, sm in list(self.sems.items()):
            if len(key) > 1 and key[0] == 'c' and key[1] == other:
                sp.sem_clear(sm)
        sp.sem_inc(self.sem(('HS',)), 1)

    def unit_done(self):
        self.units = getattr(self, 'units', 0) + 1

    def dma(self, out, in_, reads=(), writes=(), q='sp', **kw):
        self.op(q, lambda E: E.dma_start(out=out, in_=in_, **kw), reads, writes, dma=True)

    def mm(self, out, lhsT, rhs, start, stop, reads=(), writes=()):
        self.op('pe', lambda E: E.matmul(out, lhsT, rhs, start=start, stop=stop), reads, writes)

    def tr(self, out, in_, ident, reads=(), writes=()):
        self.op('pe', lambda E: E.transpose(out, in_, ident), reads, writes)

    def act(self, out, in_, func, reads=(), writes=(), **kw):
        self.op('act', lambda E: E.activation(out, in_, func, **kw), reads, writes)

    def ts(self, e, out, in0, s1, s2, op0, op1=None, reads=(), writes=(), **kw):
        if op1 is None:
            self.op(e, lambda E: E.tensor_scalar(out, in0, s1, None, op0, **kw), reads, writes)
        else:
            self.op(e, lambda E: E.tensor_scalar(out, in0, s1, s2, op0, op1, **kw), reads, writes)

    def tt(self, e, out, in0, in1, op, reads=(), writes=()):
        self.op(e, lambda E: E.tensor_tensor(out, in0, in1, op), reads, writes)

    def stt(self, e, out, in0, scalar, in1, op0, op1, reads=(), writes=()):
        self.op(e, lambda E: E.scalar_tensor_tensor(out, in0, scalar, in1, op0, op1), reads, writes)

    def copy(self, e, out, in_, reads=(), writes=()):
        if e == 'act':
            self.op(e, lambda E: E.copy(out, in_), reads, writes)
        else:
            self.op(e, lambda E: E.tensor_copy(out, in_), reads, writes)

    def memset(self, e, ap, val, writes=()):
        self.op(e, lambda E: E.memset(ap, val), (), writes)


_UNIQ = [0]


class Ring:
    def __init__(self, nc, st, name, n, shape, dtype, psum=False):
        mk = nc.psum_tensor if psum else nc.sbuf_tensor
        _UNIQ[0] += 1
        name = f"{name}_{_UNIQ[0]}_"
        self.t = [st.enter_context(mk(f"{name}{i}", shape, dtype)) for i in range(n)]
        self.name, self.n, self.i = name, n, 0

    def next(self):
        j = self.i % self.n
        self.i += 1
        return self.t[j], (self.name, j)


class K:
    def __init__(self, dbg=(), stop=None):
        self.dbg, self.stop = set(dbg), stop
        self.nc = nc = bass.Bass("TRN2", target_bir_lowering=False)
        self.top = ExitStack()
        self.S = Sched(nc, self.top)
        i = self.din
        self.xl = i("xl", [NE, 1024]); self.rope = i("rope", [NE, 64]); self.cc = i("cc", [128, 16])
        self.cst = i("cst", [128, 898]); self.sel = i("sel", [32, 4096])
        self.mod_w = i("mod_w", [2, 1024, 6144]); self.mod_b = i("mod_b", [2, 6144])
        self.n1w = i("norm1_w", [2, 1024]); self.n2w = i("norm2_w", [2, 1024]); self.fnw = i("final_norm_w", [1, 1024])
        self.w_in = i("even_w_in", [1024, 3072]); self.w_out0 = i("even_w_out", [1024, 1024])
        self.dlam = i("diff_lam", [1, 256]); self.dnw = i("diff_norm_w", [128, 1]); self.rdl = i("ret_decay_logit", [1, 8])
        self.rnw = i("ret_norm_w", [128, 1])
        self.w_qkv = i("odd_w_qkv", [1024, 1280]); self.w_out1 = i("odd_w_out", [1024, 1024]); self.sinks = i("odd_sinks", [1, 16])
        self.rw = i("router_w", [2, 1024, 32]); self.rb = i("router_b", [2, 32])
        ne = 1 if stop in ('mod', 'A', 'R', 'D', 'F') else 32
        self.w1 = i("moe_w1", [2, ne, 1024, 2048]); self.b1 = i("moe_b1", [2, 32, 128, 16])
        self.w2 = i("moe_w2", [2, ne, 1024, 1024]); self.b2 = i("moe_b2", [2, 32, 1024])
        self.out = nc.dram_tensor("out", [4096, 1024], F32, kind="ExternalOutput").ap()

    def din(self, name, shape, dt=F32):
        return self.nc.dram_tensor(name, shape, dt, kind="ExternalInput").ap()

    def scr(self, name, shape, dt):
        kind = "ExternalOutput" if name in self.dbg else "Internal"
        return self.nc.dram_tensor(name, shape, dt, kind=kind).ap()

    def sb(self, st, name, shape, dt):
        _UNIQ[0] += 1
        return st.enter_context(self.nc.sbuf_tensor(f"{name}_{_UNIQ[0]}", shape, dt))

    def ps(self, st, name, shape, dt):
        _UNIQ[0] += 1
        return st.enter_context(self.nc.psum_tensor(f"{name}_{_UNIQ[0]}", shape, dt))

    def consts(self):
        S, st = self.S, self.top
        self.cf = self.sb(st, "cf", [128, 898], F32)
        S.dma(self.cf[:], self.cst, writes=['cf'])
        self.idb = self.sb(st, "idb", [128, 128], BF16)
        S.copy('dve', self.idb[:], self.cf[:, 0:128], reads=['cf'], writes=['idb'])
        self.onesb = self.sb(st, "onesb", [128, 128], BF16)
        S.memset('dve', self.onesb[:], 1.0, writes=['onesb'])
        c = self.cf
        self.idf = c[:, 0:128]
        self.Af, self.Mf, self.Ab, self.Mb = c[:, 128:256], c[:, 256:384], c[:, 384:512], c[:, 512:640]
        self.IO1, self.IO2 = c[:, 640:768], c[:, 768:896]
        self.C1, self.C2 = c[:, 896:897], c[:, 897:898]
        self.Mfb = self.sb(st, "Mfb", [128, 128], BF16)
        self.Mbb = self.sb(st, "Mbb", [128, 128], BF16)
        S.copy('dve', self.Mfb[:], self.Mf, reads=['cf'], writes=['Mfb'])
        S.copy('dve', self.Mbb[:], self.Mb, reads=['cf'], writes=['Mbb'])

    def mod_phase(self, l, MT):
        S, nc = self.S, self.nc
        with ExitStack() as p:
            cT = self.sb(p, "cT", [128, 16], F32)
            sc = self.sb(p, "scT", [128, 16], F32)
            cB = self.sb(p, "cB", [128, 2, 8, 128], F32)
            S.dma(cT[:], self.cc, writes=['cT'])
            S.act(sc[:], cT[:], AF.Silu, reads=['cT'], writes=['sc'])
            scv = sc[:].rearrange("p (k t) -> p k t", t=2)
            for t in range(2):
                S.copy('dve', cB[:, t], scv[:, :, t:t + 1].to_broadcast([128, 8, 128]), reads=['sc'], writes=['cB'])
            wr = Ring(nc, p, "mw", 2, [128, 8, 512], F32)
            mbr = Ring(nc, p, "mb", 2, [128, 512], F32)
            pr = Ring(nc, p, "mps", 2, [128, 512], F32, psum=True)
            mw = self.mod_w[l].rearrange("(k p) n -> p k n", p=128)
            for g in range(12):
                w, wk = wr.next()
                S.dma(w[:], mw[:, :, g * 512:(g + 1) * 512], writes=[wk])
                mb, mbk = mbr.next()
                S.dma(mb[:], self.mod_b[l:l + 1, g * 512:(g + 1) * 512].partition_broadcast(128), writes=[mbk])
                for t in range(2):
                    ps, pk = pr.next()
                    for k in range(8):
                        S.mm(ps[:], cB[:, t, k, :], w[:, k, :], k == 0, k == 7, reads=[wk, 'cB'], writes=[pk])
                    S.tt('dve', MT[t][:, g * 512:(g + 1) * 512], ps[:], mb[:], ALU.add, reads=[pk, mbk], writes=[('MT', t, g)])
            nw = self.sb(p, "nw", [128, 2, 1024], F32)
            S.dma(nw[:, 0, :], self.n1w[l:l + 1, :].partition_broadcast(128), writes=['nw0'])
            S.dma(nw[:, 1, :], self.n2w[l:l + 1, :].partition_broadcast(128), writes=['nw1'])
            S.barrier()
            for t in range(2):
                for j, o in enumerate((1024, 4096)):
                    S.stt('dve', MT[t][:, o:o + 1024], MT[t][:, o:o + 1024], 1.0, nw[:, j, :], ALU.add, ALU.mult,
                          writes=[('MTs', t, j)])
            S.barrier()

    def front(self, xt, xk, S1, B1, rs_ring, tmp_ring, ab_ring, pT_ring, aT_ring, tabkeys=()):
        S = self.S
        rs, rk = rs_ring.next()
        tmp, tk = tmp_ring.next()
        S.act(tmp[:], xt, AF.Square, reads=[xk], writes=[tk, rk], accum_out=rs[:, 0:1])
        S.ts('dve', rs[:, 1:2], rs[:, 0:1], 1.0 / 1024, 1e-6, ALU.mult, ALU.add, reads=[rk], writes=[rk])
        S.op('act', lambda E: E.sqrt(rs[:, 2:3], rs[:, 1:2]), reads=[rk], writes=[rk])
        S.op('dve', lambda E: E.reciprocal(rs[:, 3:4], rs[:, 2:3]), reads=[rk], writes=[rk])
        S.stt('dve', tmp[:], xt, rs[:, 3:4], S1, ALU.mult, ALU.mult, reads=[xk, rk] + list(tabkeys), writes=[tk])
        ab, abk = ab_ring.next()
        S.tt('pool', ab[:], tmp[:], B1, ALU.add, reads=[tk] + list(tabkeys), writes=[abk])
        pT, pTk = pT_ring.next()
        for k in range(8):
            S.tr(pT[:, k, :], ab[:, k * 128:(k + 1) * 128], self.idb[:], reads=[abk, 'idb'], writes=[pTk])
        aT, aTk = aT_ring.next()
        S.copy('act', aT[:], pT[:], reads=[pTk], writes=[aTk])
        self.last_ab = (ab, abk)
        return aT, aTk

    def load_w_bf16(self, st, name, src, ncols, stage_ring):
        S = self.S
        w = self.sb(st, name, [128, 8, ncols], BF16)
        sv = src.rearrange("(k p) n -> p k n", p=128)
        for k in range(8):
            for c0 in range(0, ncols, 1024):
                c1 = min(ncols, c0 + 1024)
                stg, sk = stage_ring.next()
                S.dma(stg[:, 0:c1 - c0], sv[:, k, c0:c1], writes=[sk])
                S.copy('pool', w[:, k, c0:c1], stg[:, 0:c1 - c0], reads=[sk], writes=[(name, k, c0)])
        return w

    def l0_A(self, MT):
        S, nc = self.S, self.nc
        d = self.scr
        self.qT0 = d("qT0", [4, 128, NQ], BF16); self.kT0 = d("kT0", [4, 128, NE], BF16); self.v0 = d("v0", [NE, 512], BF16)
        self.rkf = d("rkf", [NE, 256], BF16); self.rkb = d("rkb", [NE, 256], BF16); self.rkT = d("rkT", [2, 128, NE], BF16)
        self.rv0 = d("rv0", [NE, 512], BF16)
        self.rqT = d("rqT", [3, 2, 128, NQ], BF16)
        self.sgT = d("sgT", [4, 128, NQ], BF16)
        with ExitStack() as p:
            stg = Ring(nc, p, "stg", 2, [128, 1024], F32)
            w = self.load_w_bf16(p, "win", self.w_in, 3072, stg)
            lg = self.lg
            KF = self.sb(p, "KF", [128, 2, 4], F32)
            QD = self.sb(p, "QD", [128, 2, 2, 128], F32)
            for dr in range(2):
                for h in range(4):
                    S.act(KF[:, dr, h:h + 1], self.C1 if dr == 0 else self.C2, AF.Exp, reads=['lg', 'cf'], writes=['KF'],
                          scale=lg[:, dr * 4 + h:dr * 4 + h + 1])
                    c, hh = h // 2, h % 2
                    S.act(QD[hh * 64:(hh + 1) * 64, dr, c, :], (self.IO1 if dr == 0 else self.IO2)[hh * 64:(hh + 1) * 64, :], AF.Exp,
                          reads=['lg', 'cf'], writes=['QD'], scale=lg[hh * 64:(hh + 1) * 64, dr * 4 + h:dr * 4 + h + 1])
            xr = Ring(nc, p, "xt", 3, [128, 1024], F32)
            rpr = Ring(nc, p, "rp", 3, [128, 64], F32)
            rsr = Ring(nc, p, "rs", 3, [128, 4], F32)
            tmr = Ring(nc, p, "tmp", 2, [128, 1024], F32)
            abr = Ring(nc, p, "ab", 2, [128, 1024], BF16)
            aTr = Ring(nc, p, "aT", 2, [128, 8, 128], BF16)
            pTr = Ring(nc, p, "pT", 2, [128, 8, 128], BF16, psum=True)
            pPr = Ring(nc, p, "pP", 3, [128, 512], F32, psum=True)
            pFr = Ring(nc, p, "pF", 2, [128, 4, 128], F32, psum=True)
            pQr = Ring(nc, p, "pQ", 1, [128, 4, 128], BF16, psum=True)
            r1 = Ring(nc, p, "r1", 2, [128, 4, 256], F32)
            qrr = Ring(nc, p, "qr", 2, [128, 512], BF16)
            qTs = Ring(nc, p, "qTs", 3, [128, 4, 128], BF16)
            vbr = Ring(nc, p, "vb", 3, [128, 512], BF16)
            kdr = Ring(nc, p, "kd", 3, [128, 2, 256], BF16)
            fTr = Ring(nc, p, "fTs", 3, [128, 4, 128], BF16)
            q3r = Ring(nc, p, "q3", 2, [128, 3, 2, 128], BF16)

            def tokmajor(aT, aTk, c0, n):
                ps, pk = pPr.next()
                for k in range(8):
                    S.mm(ps[:, 0:n], aT[:, k, :], w[:, k, c0:c0 + n], k == 0, k == 7, reads=[aTk], writes=[pk])
                return ps, pk

            def featmajor(aT, aTk, c0, nch):
                ps, pk = pFr.next()
                for j in range(nch):
                    for k in range(8):
                        S.mm(ps[:, j, :], w[:, k, c0 + j * 128:c0 + (j + 1) * 128], aT[:, k, :], k == 0, k == 7, reads=[aTk], writes=[pk])
                return ps, pk

            def rope_T(ps, pk, rp, rpk, dst, col0):
                xv = ps[:].rearrange("p (h r a f) -> p h r a f", h=8, r=2, a=2, f=16)
                a, b = xv[:, :, :, 0, :], xv[:, :, :, 1, :]
                cos = rp[:, 0:32].rearrange("p (r f) -> p r f", r=2).unsqueeze(1).to_broadcast([128, 8, 2, 16])
                sin = rp[:, 32:64].rearrange("p (r f) -> p r f", r=2).unsqueeze(1).to_broadcast([128, 8, 2, 16])
                t, tk = r1.next()
                tv = [t[:, i, :].rearrange("p (h r f) -> p h r f", h=8, r=2) for i in range(4)]
                S.tt('dve', tv[0], a, cos, ALU.mult, reads=[pk, rpk], writes=[(tk, 0)])
                S.tt('dve', tv[1], b, sin, ALU.mult, reads=[pk, rpk], writes=[(tk, 1)])
                S.tt('dve', tv[2], a, sin, ALU.mult, reads=[pk, rpk], writes=[(tk, 2)])
                S.tt('dve', tv[3], b, cos, ALU.mult, reads=[pk, rpk], writes=[(tk, 3)])
                qr, qk = qrr.next()
                qv = qr[:].rearrange("p (h r a f) -> p h r a f", h=8, r=2, a=2, f=16)
                S.tt('pool', qv[:, :, :, 0, :], tv[0], tv[1], ALU.subtract, reads=[(tk, 0), (tk, 1)], writes=[(qk, 0)])
                S.tt('pool', qv[:, :, :, 1, :], tv[2], tv[3], ALU.add, reads=[(tk, 2), (tk, 3)], writes=[(qk, 1)])
                pq, pqk = pQr.next()
                for h in range(4):
                    S.tr(pq[:, h, :], qr[:, h * 128:(h + 1) * 128], self.idb[:], reads=[(qk, 0), (qk, 1)], writes=[pqk])
                qs, qsk = qTs.next()
                S.copy('act', qs[:], pq[:], reads=[pqk], writes=[qsk])
                S.dma(dst[:, :, col0:col0 + 128].rearrange("h p n -> p h n"), qs[:], reads=[qsk], writes=[], q='pool')

            KLIM = int(os.environ.get('KLIM', NT_ALL)); KCUT = int(os.environ.get('KCUT', 99))
            for i in range(min(NT_ALL, KLIM)):
                t = 1 if i < 2 else 0
                needq = i < NT_Q
                r0 = i * 128
                xt, xk = xr.next()
                S.dma(xt[:], self.xl[r0:r0 + 128, :], writes=[xk])
                rp, rpk = rpr.next()
                S.dma(rp[:], self.rope[r0:r0 + 128, :], writes=[rpk])
                aT, aTk = self.front(xt[:], xk, MT[t][:, 1024:2048], MT[t][:, 0:1024], rsr, tmr, abr, pTr, aTr)
                if KCUT < 1: continue
                ps, pk = tokmajor(aT, aTk, 512, 512)
                rope_T(ps, pk, rp, rpk, self.kT0, r0)
                if needq:
                    ps, pk = tokmajor(aT, aTk, 0, 512)
                    rope_T(ps, pk, rp, rpk, self.qT0, r0)
                if KCUT < 2: continue
                ps, pk = tokmajor(aT, aTk, 1024, 512)
                vb, vk = vbr.next()
                S.copy('act', vb[:], ps[:], reads=[pk], writes=[vk])
                S.dma(self.v0[r0:r0 + 128, :], vb[:], reads=[vk], q='pool')
                ps, pk = tokmajor(aT, aTk, 2048, 512)
                vb, vk = vbr.next()
                S.copy('act', vb[:], ps[:], reads=[pk], writes=[vk])
                S.dma(self.rv0[r0:r0 + 128, :], vb[:], reads=[vk], q='pool')
                if KCUT < 3: continue
                ps, pk = tokmajor(aT, aTk, 1792, 256)
                kd, kk = kdr.next()
                for dr in range(2):
                    S.tt('dve', kd[:, dr, :].rearrange("p (h d) -> p h d", h=4), ps[:, 0:256].rearrange("p (h d) -> p h d", h=4),
                         KF[:, dr, :].unsqueeze(2).to_broadcast([128, 4, 64]), ALU.mult, reads=[pk, 'KF'], writes=[(kk, dr)])
                S.dma(self.rkf[r0:r0 + 128, :], kd[:, 0, :], reads=[(kk, 0)], q='pool')
                S.dma(self.rkb[r0:r0 + 128, :], kd[:, 1, :], reads=[(kk, 1)], q='pool')
                if KCUT < 4: continue
                ps, pk = featmajor(aT, aTk, 1792, 2)
                fT, fk = fTr.next()
                S.copy('act', fT[:, 0:2, :], ps[:, 0:2, :], reads=[pk], writes=[fk])
                S.dma(self.rkT[:, :, r0:r0 + 128].rearrange("c p n -> p c n"), fT[:, 0:2, :], reads=[fk], q='pool')
                if KCUT < 5: continue
                if needq:
                    ps, pk = featmajor(aT, aTk, 1536, 2)
                    q3, q3k = q3r.next()
                    S.op('act', lambda E: E.mul(q3[:, 0, :, :], ps[:, 0:2, :], 0.125), reads=[pk], writes=[(q3k, 0), pk])
                    for dr in range(2):
                        S.stt('dve', q3[:, 1 + dr, :, :], ps[:, 0:2, :], 0.125, QD[:, dr, :, :], ALU.mult, ALU.mult,
                              reads=[pk, 'QD'], writes=[(q3k, 1 + dr), pk])
                    for v in range(3):
                        S.dma(self.rqT[v, :, :, r0:r0 + 128].rearrange("c p n -> p c n"), q3[:, v, :, :], reads=[(q3k, v)], q='pool')
                    if KCUT < 6: continue
                    ps, pk = featmajor(aT, aTk, 2560, 4)
                    fT, fk = fTr.next()
                    S.act(fT[:], ps[:], AF.Silu, reads=[pk], writes=[fk])
                    S.dma(self.sgT[:, :, r0:r0 + 128].rearrange("c p n -> p c n"), fT[:], reads=[fk], q='pool')
            S.barrier()


    def ret_lg(self, st):
        S = self.S
        dl = self.sb(st, "dl", [128, 8], F32)
        S.dma(dl[:], self.rdl.partition_broadcast(128), writes=['dl'])
        self.lg = lg = self.sb(st, "lg", [128, 8], F32)
        S.act(lg[:], dl[:], AF.Exp, reads=['dl'], writes=['lg'], scale=-1.0)
        S.ts('dve', lg[:], lg[:], 1.0, None, ALU.add, reads=['lg'], writes=['lg'])
        S.act(lg[:], lg[:], AF.Ln, reads=['lg'], writes=['lg'])
        S.ts('dve', lg[:], lg[:], -1.0, None, ALU.mult, reads=['lg'], writes=['lg'])
        self.G128 = g = self.sb(st, "G128", [128, 8], F32)
        S.act(g[:], lg[:], AF.Exp, reads=['lg'], writes=['G128'], scale=128.0)
        la = self.sb(st, "la", [128, 256], F32)
        S.dma(la[:], self.dlam.partition_broadcast(128), writes=['la'])
        lv = la[:].rearrange("p (q two d) -> p q two d", q=2, two=2)
        pr = self.sb(st, "lapr", [128, 2, 64], F32)
        S.tt('dve', pr[:], lv[:, :, 0, :], lv[:, :, 1, :], ALU.mult, reads=['la'], writes=['lapr'])
        sm = self.sb(st, "lasm", [128, 4], F32)
        S.op('dve', lambda E: E.reduce_sum(sm[:, 0:2], pr[:], AX.X), reads=['lapr'], writes=['lasm'])
        S.act(sm[:, 2:4], sm[:, 0:2], AF.Exp, reads=['lasm'], writes=['lasm'])
        self.nlam = nl = self.sb(st, "nlam", [128, 1], F32)
        S.tt('dve', nl[:], sm[:, 3:4], sm[:, 2:3], ALU.subtract, reads=['lasm'], writes=['nlam'])
        S.ts('dve', nl[:], nl[:], -0.2, None, ALU.add, reads=['nlam'], writes=['nlam'])
        S.barrier()

    def l0_R(self):
        S, nc = self.S, self.nc
        self.drT = self.scr("drT", [8, 128, NQ], BF16)
        lg = self.lg
        with ExitStack() as p:
            Ds = self.sb(p, "Ds", [128, 4, 128], F32)
            dtmp = self.sb(p, "dtmp", [128, 2, 128], F32)
            for h in range(4):
                S.act(dtmp[:, 0, :], self.Af, AF.Exp, reads=['cf', 'lg'], writes=['dtmp0'], scale=lg[:, h:h + 1])
                S.act(dtmp[:, 1, :], self.Ab, AF.Exp, reads=['cf', 'lg'], writes=['dtmp1'], scale=lg[:, 4 + h:5 + h])
                S.tt('dve', dtmp[:, 0, :], dtmp[:, 0, :], self.Mf, ALU.mult, reads=['dtmp0'], writes=['dtmp0'])
                S.tt('dve', dtmp[:, 1, :], dtmp[:, 1, :], self.Mb, ALU.mult, reads=['dtmp1'], writes=['dtmp1'])
                S.tt('dve', Ds[:, h, :], dtmp[:, 0, :], dtmp[:, 1, :], ALU.add, reads=['dtmp0', 'dtmp1'], writes=['Ds'])
            rnw = self.sb(p, "rnw", [128, 1], F32)
            S.dma(rnw[:], self.rnw, writes=['rnw'])
            onesf = self.sb(p, "onesf", [128, 128], F32)
            S.memset('dve', onesf[:], 1.0, writes=['onesf'])
            Sb = self.sb(p, "Sb", [128, 2, NT_Q, 2, 128], BF16)
            Sm = Ring(nc, p, "Sm", 2, [128, 2, 128], F32)
            kdr = Ring(nc, p, "rkd", 3, [128, 256], BF16)
            vr = Ring(nc, p, "rvv", 3, [128, 512], BF16)
            pkv = Ring(nc, p, "pkv", 2, [128, 4, 128], F32, psum=True)
            fwd = list(range(NT_Q))
            bwd = [1, 0] + list(range(NT_ALL - 1, 1, -1))
            for dr, order, src in ((0, fwd, self.rkf), (1, bwd, self.rkb)):
                cur, ck = Sm.next()
                S.memset('dve', cur[:], 0.0, writes=[ck])
                for n in order:
                    if n < NT_Q:
                        S.copy('act', Sb[:, dr, n, :, :], cur[:], reads=[ck], writes=[('Sb', dr, n)])
                    if n == order[-1]:
                        break
                    kd, kk = kdr.next()
                    S.dma(kd[:], src[n * 128:(n + 1) * 128, :], writes=[kk])
                    v, vk = vr.next()
                    S.dma(v[:], self.rv0[n * 128:(n + 1) * 128, :], writes=[vk])
                    ps, pk = pkv.next()
                    for h in range(4):
                        c = h // 2
                        S.mm(ps[:, h, :], kd[:, c * 128:(c + 1) * 128], v[:, h * 128:(h + 1) * 128], True, True, reads=[kk, vk], writes=[pk])
                    nxt, nk = Sm.next()
                    for h in range(4):
                        c, hh = h // 2, h % 2
                        sl = slice(hh * 64, (hh + 1) * 64)
                        S.stt('dve', nxt[sl, c, :], cur[sl, c, :], self.G128[sl, dr * 4 + h:dr * 4 + h + 1], ps[sl, h, :], ALU.mult, ALU.add,
                              reads=[ck, pk, 'G128'], writes=[nk])
                    cur, ck = nxt, nk
            ktr = Ring(nc, p, "rkt", 2, [128, 2, 128], BF16)
            qr = Ring(nc, p, "rq3", 2, [128, 3, 2, 128], BF16)
            sgr = Ring(nc, p, "rsg", 2, [128, 4, 128], BF16)
            pTr = Ring(nc, p, "rpT", 3, [128, 128], BF16)
            pS = Ring(nc, p, "pS", 2, [128, 128], F32, psum=True)
            pO = Ring(nc, p, "pO", 2, [128, 4, 128], F32, psum=True)
            pN = Ring(nc, p, "pN", 1, [128, 512], F32, psum=True)
            osr = Ring(nc, p, "ros", 2, [128, 512], F32)
            sqr = Ring(nc, p, "rsq", 2, [128, 512], F32)
            rsr = Ring(nc, p, "rrs", 2, [128, 512], F32)
            rtr = Ring(nc, p, "rrt", 2, [128, 4, 128], BF16)
            for n in range(NT_Q):
                c0 = n * 128
                kt, ktk = ktr.next()
                S.dma(kt[:], self.rkT[:, :, c0:c0 + 128].rearrange("c p n -> p c n"), writes=[ktk])
                q3, qk = qr.next()
                for v_ in range(3):
                    S.dma(q3[:, v_, :, :], self.rqT[v_, :, :, c0:c0 + 128].rearrange("c p n -> p c n"), writes=[(qk, v_)])
                v, vk = vr.next()
                S.dma(v[:], self.rv0[c0:c0 + 128, :], writes=[vk])
                sg, sgk = sgr.next()
                S.dma(sg[:], self.sgT[:, :, c0:c0 + 128].rearrange("c p n -> p c n"), writes=[sgk])
                po, pok = pO.next()
                for h in range(4):
                    c, hh = h // 2, h % 2
                    sl = slice(hh * 64, (hh + 1) * 64)
                    ps, psk = pS.next()
                    S.mm(ps[:], kt[sl, c, :], q3[sl, 0, c, :], True, True, reads=[ktk, (qk, 0)], writes=[psk])
                    pT, pTk = pTr.next()
                    S.tt('dve', pT[:], ps[:], Ds[:, h, :], ALU.mult, reads=[psk, 'Ds'], writes=[pTk])
                    S.mm(po[:, h, :], v[:, h * 128:(h + 1) * 128], pT[:], True, False, reads=[vk, pTk], writes=[pok])
                    S.mm(po[:, h, :], Sb[sl, 0, n, c, :], q3[sl, 1, c, :], False, False, reads=[('Sb', 0, n), (qk, 1)], writes=[pok])
                    S.mm(po[:, h, :], Sb[sl, 1, n, c, :], q3[sl, 2, c, :], False, True, reads=[('Sb', 1, n), (qk, 2)], writes=[pok])
                osb, ok = osr.next()
                S.copy('act', osb[:], po[:].rearrange("p h n -> p (h n)"), reads=[pok], writes=[ok])
                sq, sqk = sqr.next()
                S.tt('pool', sq[:], osb[:], osb[:], ALU.mult, reads=[ok], writes=[sqk])
                pn, pnk = pN.next()
                S.mm(pn[:], onesf[:], sq[:], True, True, reads=[sqk, 'onesf'], writes=[pnk])
                rs, rk = rsr.next()
                S.ts('dve', rs[:], pn[:], 1.0 / 128, 1e-6, ALU.mult, ALU.add, reads=[pnk], writes=[rk])
                S.op('act', lambda E: E.sqrt(rs[:], rs[:]), reads=[rk], writes=[rk])
                S.op('dve', lambda E: E.reciprocal(rs[:], rs[:]), reads=[rk], writes=[rk])
                S.stt('dve', osb[:], osb[:], rnw[:, 0:1], rs[:], ALU.mult, ALU.mult, reads=[ok, rk, 'rnw'], writes=[ok])
                rt, rtk = rtr.next()
                S.tt('pool', rt[:].rearrange("p h n -> p (h n)"), osb[:], sg[:].rearrange("p h n -> p (h n)"), ALU.mult, reads=[ok, sgk], writes=[rtk])
                S.dma(self.drT[4:8, :, c0:c0 + 128].rearrange("c p n -> p c n"), rt[:], reads=[rtk], q='pool')
            S.barrier()

    def l0_D(self):
        S, nc = self.S, self.nc
        with ExitStack() as p:
            dn8 = self.sb(p, "dn8", [128, 1], F32)
            S.dma(dn8[:], self.dnw, writes=['dn8'])
            S.ts('dve', dn8[:], dn8[:], 0.8, None, ALU.mult, reads=['dn8'], writes=['dn8'])
            onesf = self.sb(p, "onesf2", [128, 128], F32)
            S.memset('dve', onesf[:], 1.0, writes=['onesf'])
            KTr = Ring(nc, p, "dKT", 2, [128, NE], BF16)
            Vr = Ring(nc, p, "dV", 2, [128, NT_ALL, 128], BF16)
            QTr = Ring(nc, p, "dQT", 2, [128, 512], BF16)
            pSr = Ring(nc, p, "dpS", 4, [128, 512], F32, psum=True)
            pO = [self.ps(p, f"dpO{c}", [128, 512], F32) for c in range(2)]
            pZ = [self.ps(p, f"dpZ{c}", [128, 512], F32) for c in range(2)]
            pTr = Ring(nc, p, "dpT", 5, [128, 512], BF16)
            zacc = [self.sb(p, f"dzacc{c}", [128, 512], F32) for c in range(2)]
            rzr = Ring(nc, p, "drz", 2, [128, 512], F32)
            tr_ = Ring(nc, p, "dtt", 3, [128, 512], F32)
            dor = Ring(nc, p, "ddo", 2, [128, 512], BF16)
            tiles = [(0, 256, [0, 1])] + [(256 + 512 * t, 512, list(range(NT_ALL))) for t in range(8)] + [(4352, 128, list(range(NT_ALL)))]
            QLIM = int(os.environ.get('QLIM', 99))
            tiles = tiles[:QLIM]
            for h in range(4):
                kt, ktk = KTr.next()
                S.dma(kt[:], self.kT0[h], writes=[ktk])
                v, vk = Vr.next()
                vsrc = self.v0[:, h * 128:(h + 1) * 128].rearrange("(c p) e -> p c e", p=128)
                for g in range(0, NT_ALL, 11):
                    S.dma(v[:, g:g + 11, :], vsrc[:, g:g + 11, :], writes=[(vk, g)])
                vkeys = [(vk, g) for g in range(0, NT_ALL, 11)]
                for (q0, N, chunks) in tiles:
                    qt, qk_ = QTr.next()
                    S.dma(qt[:, 0:N], self.qT0[h][:, q0:q0 + N], writes=[qk_])
                    last = len(chunks) - 1
                    units = [(idx, kc, c) for idx, kc in enumerate(chunks) for c in range(2)]
                    pend = []

                    def qk(u):
                        idx, kc, c = u
                        sl = slice(c * 64, (c + 1) * 64)
                        ps, psk = pSr.next()
                        S.mm(ps[:, 0:N], kt[sl, kc * 128:(kc + 1) * 128], qt[sl, 0:N], True, True, reads=[ktk, qk_], writes=[psk])
                        pT, pTk = pTr.next()
                        S.act(pT[:, 0:N], ps[:, 0:N], AF.Exp, reads=[psk], writes=[pTk], scale=0.125)
                        pend.append((u, pT, pTk))

                    def pv():
                        (idx, kc, c), pT, pTk = pend.pop(0)
                        S.mm(pO[c][:, 0:N], v[:, kc, :], pT[:, 0:N], idx == 0, idx == last, reads=[pTk] + vkeys, writes=[('dO', c)])
                        if idx == 0:
                            S.copy('dve', zacc[c][:, 0:N], pT[:, 0:N], reads=[pTk], writes=[('zacc', c)])
                        else:
                            S.tt('dve', zacc[c][:, 0:N], zacc[c][:, 0:N], pT[:, 0:N], ALU.add, reads=[pTk, ('zacc', c)], writes=[('zacc', c)])
                        if idx == last:
                            S.mm(pZ[c][:, 0:N], onesf[:], zacc[c][:, 0:N], True, True, reads=[('zacc', c), 'onesf'], writes=[('dZ', c)])

                    LOOK = 3
                    for ui, u in enumerate(units):
                        qk(u)
                        if ui >= LOOK:
                            pv()
                    while pend:
                        pv()
                    tt = []
                    for c in range(2):
                        rz, rzk = rzr.next()
                        S.op('dve', lambda E: E.reciprocal(rz[:, 0:N], pZ[c][:, 0:N]), reads=[('dZ', c)], writes=[rzk, ('dZ', c)])
                        t_, tk = tr_.next()
                        S.tt('dve', t_[:, 0:N], pO[c][:, 0:N], rz[:, 0:N], ALU.mult, reads=[('dO', c), rzk], writes=[tk, ('dO', c)])
                        tt.append((t_, tk))
                    o, ok = tr_.next()
                    S.stt('dve', o[:, 0:N], tt[1][0][:, 0:N], self.nlam[:, 0:1], tt[0][0][:, 0:N], ALU.mult, ALU.add,
                          reads=[tt[0][1], tt[1][1], 'nlam'], writes=[ok])
                    sq, sqk = rzr.next()
                    S.tt('pool', sq[:, 0:N], o[:, 0:N], o[:, 0:N], ALU.mult, reads=[ok], writes=[sqk])
                    pN, pNk = pSr.next()
                    S.mm(pN[:, 0:N], onesf[:], sq[:, 0:N], True, True, reads=[sqk, 'onesf'], writes=[pNk])
                    rs, rk = rzr.next()
                    S.ts('dve', rs[:, 0:N], pN[:, 0:N], 1.0 / 128, 1e-6, ALU.mult, ALU.add, reads=[pNk], writes=[rk, pNk])
                    S.op('act', lambda E: E.sqrt(rs[:, 0:N], rs[:, 0:N]), reads=[rk], writes=[rk])
                    S.op('dve', lambda E: E.reciprocal(rs[:, 0:N], rs[:, 0:N]), reads=[rk], writes=[rk])
                    do, dk = dor.next()
                    S.stt('dve', do[:, 0:N], o[:, 0:N], dn8[:, 0:1], rs[:, 0:N], ALU.mult, ALU.mult, reads=[ok, rk, 'dn8'], writes=[dk])
                    S.dma(self.drT[h][:, q0:q0 + N], do[:, 0:N], reads=[dk], q='pool')
            S.barrier()

    def post_init(self, p, l, ntok):
        S, nc = self.S, self.nc
        R = {}
        R['rwb'] = rwb = self.sb(p, "rwb", [128, 8, 32], BF16)
        rwf = self.sb(p, "rwf", [128, 8, 32], F32)
        S.dma(rwf[:], self.rw[l].rearrange("(k p) e -> p k e", p=128), writes=['rwf'])
        S.copy('dve', rwb[:], rwf[:], reads=['rwf'], writes=['rwb'])
        R['rbt'] = rbt = self.sb(p, "rbt", [128, 32], F32)
        S.dma(rbt[:], self.rb[l:l + 1, :].partition_broadcast(128), writes=['rbt'])
        R['xr'] = Ring(nc, p, "pxt", 2, [128, 1024], F32)
        R['tm'] = Ring(nc, p, "ptm", 2, [128, 1024], F32)
        R['h1'] = Ring(nc, p, "ph1", 2, [128, 1024], F32)
        R['rs'] = Ring(nc, p, "prs", 3, [128, 4], F32)
        R['tmp'] = Ring(nc, p, "ptmp", 2, [128, 1024], F32)
        R['ab'] = Ring(nc, p, "pab", 2, [128, 1024], BF16)
        R['aT'] = Ring(nc, p, "paT", 3, [128, 8, 128], BF16)
        R['pT'] = Ring(nc, p, "ppT", 1, [128, 8, 128], BF16, psum=True)
        R['pL'] = Ring(nc, p, "ppL", 1, [128, 512], F32, psum=True)
        R['lg'] = Ring(nc, p, "plg", 2, [128, 32], F32)
        R['sm'] = Ring(nc, p, "psm", 2, [128, 16], F32)
        R['eg'] = Ring(nc, p, "peg", 2, [128, 3, 32], F32)
        R['gt'] = Ring(nc, p, "pgt", 2, [32, 128], F32)
        return R

    def post_mixer(self, R, i, r0, t, MT, mps, hsrc, h1d, fTd, gTd, fTokd=None, gTokd=None):
        S = self.S
        xt, xk = R['xr'].next()
        S.dma(xt[:], hsrc, writes=[xk])
        tm, tmk = R['tm'].next()
        for n in range(2):
            S.tt('dve', tm[:, n * 512:(n + 1) * 512], mps[n][0][:], MT[t][:, 2048 + n * 512:2048 + (n + 1) * 512], ALU.mult,
                 reads=[mps[n][1]], writes=[(tmk, n), mps[n][1]])
        h1, h1k = R['h1'].next()
        S.tt('pool', h1[:], tm[:], xt[:], ALU.add, reads=[(tmk, 0), (tmk, 1), xk], writes=[h1k])
        S.dma(h1d[r0:r0 + 128, :], h1[:], reads=[h1k], q='pool')
        aT, aTk = self.front(h1[:], h1k, MT[t][:, 4096:5120], MT[t][:, 3072:4096], R['rs'], R['tmp'], R['ab'], R['pT'], R['aT'])
        S.dma(fTd[:, :, r0:r0 + 128].rearrange("k p n -> p k n"), aT[:], reads=[aTk], q='pool')
        if fTokd is not None:
            ab, abk = self.last_ab
            S.dma(fTokd[r0:r0 + 128, :], ab[:], reads=[abk], q='pool')
        pl, plk = R['pL'].next()
        for k in range(8):
            S.mm(pl[:, 0:32], aT[:, k, :], R['rwb'][:, k, :], k == 0, k == 7, reads=[aTk, 'rwb'], writes=[plk])
        lg, lgk = R['lg'].next()
        S.tt('dve', lg[:], pl[:, 0:32], R['rbt'][:], ALU.add, reads=[plk, 'rbt'], writes=[lgk, plk])
        sm, smk = R['sm'].next()
        S.op('dve', lambda E: E.max(sm[:, 0:8], lg[:]), reads=[lgk], writes=[smk])
        eg, egk = R['eg'].next()
        S.ts('dve', eg[:, 0, :], lg[:], sm[:, 3:4], None, ALU.is_ge, reads=[lgk, smk], writes=[(egk, 0)])
        S.ts('dve', sm[:, 8:9], sm[:, 0:1], -1.0, None, ALU.mult, reads=[smk], writes=[smk])
        S.act(eg[:, 1, :], lg[:], AF.Exp, reads=[lgk, smk], writes=[(egk, 1)], bias=sm[:, 8:9])
        S.tt('dve', eg[:, 1, :], eg[:, 1, :], eg[:, 0, :], ALU.mult, reads=[(egk, 0), (egk, 1)], writes=[(egk, 1)])
        S.op('dve', lambda E: E.reduce_sum(sm[:, 9:10], eg[:, 1, :], AX.X), reads=[(egk, 1), smk], writes=[smk])
        S.op('dve', lambda E: E.reciprocal(sm[:, 10:11], sm[:, 9:10]), reads=[smk], writes=[smk])
        S.ts('dve', eg[:, 2, :], eg[:, 1, :], sm[:, 10:11], None, ALU.mult, reads=[(egk, 1), smk], writes=[(egk, 2)])
        if gTokd is not None:
            S.dma(gTokd[r0:r0 + 128, :], eg[:, 2, :], reads=[(egk, 2)], q='pool')
        pg, pgk = R['pL'].next()
        S.tr(pg[0:32, 0:128], eg[:, 2, :], self.idf, reads=[(egk, 2), 'cf'], writes=[pgk])
        gt, gtk = R['gt'].next()
        S.copy('act', gt[:], pg[0:32, 0:128], reads=[pgk], writes=[gtk, pgk])
        S.dma(gTd[:, r0:r0 + 128], gt[:], reads=[gtk], q='pool')

    def l0_F(self, MT):
        S, nc = self.S, self.nc
        self.h1 = self.scr("h1", [NQ, 1024], F32)
        self.fT0 = self.scr("fT0", [8, 128, NQ], BF16)
        self.gT0 = self.scr("gT0", [32, NQ], F32)
        self.fTok0 = self.scr("fTok0", [NQ, 1024], BF16)
        self.gTok0 = self.scr("gTok0", [NQ, 32], F32)
        with ExitStack() as p:
            stg = Ring(nc, p, "stgF", 2, [128, 1024], F32)
            wo = self.load_w_bf16(p, "wo0", self.w_out0, 1024, stg)
            R = self.post_init(p, 0, NQ)
            drr = Ring(nc, p, "fdr", 3, [128, 8, 128], BF16)
            pM = Ring(nc, p, "fpM", 4, [128, 512], F32, psum=True)
            for i in range(NT_Q):
                r0 = i * 128
                t = 1 if i < 2 else 0
                dr, drk = drr.next()
                S.dma(dr[:], self.drT[:, :, r0:r0 + 128].rearrange("k p n -> p k n"), writes=[drk])
                mps = []
                for n in range(2):
                    ps, pk = pM.next()
                    for k in range(8):
                        S.mm(ps[:], dr[:, k, :], wo[:, k, n * 512:(n + 1) * 512], k == 0, k == 7, reads=[drk], writes=[pk])
                    mps.append((ps, pk))
                self.post_mixer(R, i, r0, t, MT, mps, self.xl[r0:r0 + 128, :], self.h1, self.fT0, self.gT0, self.fTok0, self.gTok0)
            S.barrier()

    def rope_tm(self, src, srck, H, rp, rpk, r1, dst, dstk):
        S = self.S
        xv = src.rearrange("p (h r a f) -> p h r a f", h=H, r=2, a=2, f=16)
        a, b = xv[:, :, :, 0, :], xv[:, :, :, 1, :]
        cos = rp[:, 0:32].rearrange("p (r f) -> p r f", r=2).unsqueeze(1).to_broadcast([128, H, 2, 16])
        sin = rp[:, 32:64].rearrange("p (r f) -> p r f", r=2).unsqueeze(1).to_broadcast([128, H, 2, 16])
        t, tk = r1.next()
        tv = [t[:, i, 0:H * 32].rearrange("p (h r f) -> p h r f", h=H, r=2) for i in range(4)]
        S.tt('dve', tv[0], a, cos, ALU.mult, reads=[srck, rpk], writes=[(tk, 0)])
        S.tt('dve', tv[1], b, sin, ALU.mult, reads=[srck, rpk], writes=[(tk, 1)])
        S.tt('dve', tv[2], a, sin, ALU.mult, reads=[srck, rpk], writes=[(tk, 2)])
        S.tt('dve', tv[3], b, cos, ALU.mult, reads=[srck, rpk], writes=[(tk, 3)])
        qv = dst.rearrange("p (h r a f) -> p h r a f", h=H, r=2, a=2, f=16)
        S.tt('pool', qv[:, :, :, 0, :], tv[0], tv[1], ALU.subtract, reads=[(tk, 0), (tk, 1)], writes=[(dstk, 0)])
        S.tt('pool', qv[:, :, :, 1, :], tv[2], tv[3], ALU.add, reads=[(tk, 2), (tk, 3)], writes=[(dstk, 1)])

    def l1_A(self, MT):
        S, nc = self.S, self.nc
        self.qT1 = self.scr("qT1", [8, 128, 4096], BF16)
        self.kTd1 = self.scr("kTd1", [2, 128, NQ], BF16)
        self.v1 = self.scr("v1", [NQ, 128], BF16)
        with ExitStack() as p:
            stg = Ring(nc, p, "stg1", 2, [128, 1024], F32)
            w = self.load_w_bf16(p, "wqkv", self.w_qkv, 1280, stg)
            xr = Ring(nc, p, "axt", 3, [128, 1024], F32)
            rpr = Ring(nc, p, "arp", 3, [128, 64], F32)
            rsr = Ring(nc, p, "ars", 3, [128, 4], F32)
            tmr = Ring(nc, p, "atmp", 2, [128, 1024], F32)
            abr = Ring(nc, p, "aab", 2, [128, 1024], BF16)
            aTr = Ring(nc, p, "aaT", 2, [128, 8, 128], BF16)
            pTr = Ring(nc, p, "apT", 2, [128, 8, 128], BF16, psum=True)
            pPr = Ring(nc, p, "apP", 3, [128, 512], F32, psum=True)
            pQr = Ring(nc, p, "apQ", 2, [128, 8, 128], BF16, psum=True)
            r1 = Ring(nc, p, "ar1", 2, [128, 4, 256], F32)
            qrr = Ring(nc, p, "aqr", 2, [128, 1024], BF16)
            krr = Ring(nc, p, "akr", 2, [128, 128], BF16)
            kdr = Ring(nc, p, "akd", 2, [128, 2, 128], BF16)
            qTs = Ring(nc, p, "aqT", 2, [128, 8, 128], BF16)
            kTs = Ring(nc, p, "akT", 2, [128, 2, 128], BF16)
            vbr = Ring(nc, p, "avb", 2, [128, 128], BF16)
            for i in range(NT_Q):
                t = 1 if i < 2 else 0
                r0 = i * 128
                xt, xk = xr.next()
                S.dma(xt[:], self.h2[r0:r0 + 128, :], writes=[xk])
                rp, rpk = rpr.next()
                S.dma(rp[:], self.rope[r0:r0 + 128, :], writes=[rpk])
                aT, aTk = self.front(xt[:], xk, MT[t][:, 1024:2048], MT[t][:, 0:1024], rsr, tmr, abr, pTr, aTr)
                ps, pk = pPr.next()
                for k in range(8):
                    S.mm(ps[:, 0:256], aT[:, k, :], w[:, k, 1024:1280], k == 0, k == 7, reads=[aTk], writes=[pk])
                kr, krk = krr.next()
                self.rope_tm(ps[:, 0:128], pk, 2, rp, rpk, r1, kr[:], krk)
                vb, vk = vbr.next()
                S.copy('act', vb[:], ps[:, 128:256], reads=[pk], writes=[vk, pk])
                S.dma(self.v1[r0:r0 + 128, :], vb[:], reads=[vk], q='pool')
                kd, kdk = kdr.next()
                for kvh in range(2):
                    S.copy('pool', kd[:, kvh, :].rearrange("p (two d) -> p two d", two=2),
                           kr[:, kvh * 64:(kvh + 1) * 64].unsqueeze(1).to_broadcast([128, 2, 64]), reads=[(krk, 0), (krk, 1)], writes=[(kdk, kvh)])
                pq, pqk = pQr.next()
                for kvh in range(2):
                    S.tr(pq[:, kvh, :], kd[:, kvh, :], self.idb[:], reads=[(kdk, kvh)], writes=[pqk])
                kT, kTk = kTs.next()
                S.copy('act', kT[:], pq[:, 0:2, :], reads=[pqk], writes=[kTk, pqk])
                S.dma(self.kTd1[:, :, r0:r0 + 128].rearrange("c p n -> p c n"), kT[:], reads=[kTk], q='pool')
                if 2 <= i < 34:
                    qr, qrk = qrr.next()
                    for g in range(2):
                        ps, pk = pPr.next()
                        for k in range(8):
                            S.mm(ps[:], aT[:, k, :], w[:, k, g * 512:(g + 1) * 512], k == 0, k == 7, reads=[aTk], writes=[pk])
                        self.rope_tm(ps[:], pk, 8, rp, rpk, r1, qr[:, g * 512:(g + 1) * 512], (qrk, g))
                    pq, pqk = pQr.next()
                    for c in range(8):
                        S.tr(pq[:, c, :], qr[:, c * 128:(c + 1) * 128], self.idb[:], reads=[((qrk, c // 4), 0), ((qrk, c // 4), 1)], writes=[pqk])
                    qT, qTk = qTs.next()
                    S.copy('act', qT[:], pq[:], reads=[pqk], writes=[qTk, pqk])
                    q0 = (i - 2) * 128
                    S.dma(self.qT1[:, :, q0:q0 + 128].rearrange("c p n -> p c n"), qT[:], reads=[qTk], q='pool')
            S.barrier()

    def l1_W(self, MT):
        S, nc = self.S, self.nc
        self.h3 = self.scr("h3", [4096, 1024], F32)
        self.fT1 = self.scr("fT1", [8, 128, 4096], BF16)
        self.gT1 = self.scr("gT1", [32, 4096], F32)
        self.fTok1 = self.scr("fTok1", [4096, 1024], BF16)
        self.gTok1 = self.scr("gTok1", [4096, 32], F32)
        with ExitStack() as p:
            wo = self.sb(p, "wo1", [64, 16, 1024], BF16)
            stg = Ring(nc, p, "stgW", 2, [64, 1024], F32)
            wv = self.w_out1.rearrange("(h e) n -> e h n", e=64)
            for g in range(16):
                st_, sk = stg.next()
                S.dma(st_[:], wv[:, g, :], writes=[sk])
                S.copy('pool', wo[:, g, :], st_[:], reads=[sk], writes=[('wo1x', g)])
            S.barrier()
            KT = self.sb(p, "wKT", [128, 2, NQ], BF16)
            S.dma(KT[:], self.kTd1.rearrange("c p n -> p c n"), writes=['wKT'])
            V = self.sb(p, "wV", [128, NT_Q, 128], BF16)
            S.dma(V[:], self.v1.rearrange("(c p) e -> p c e", p=128), writes=['wV'])
            ES = self.sb(p, "wES", [128, 16], F32)
            S.dma(ES[:], self.sinks.partition_broadcast(128), writes=['wES'])
            S.act(ES[:], ES[:], AF.Exp, reads=['wES'], writes=['wES'])
            S.barrier()
            R = self.post_init(p, 1, 4096)
            QTr = Ring(nc, p, "wQT", 2, [128, 8, 128], BF16)
            pS = Ring(nc, p, "wpS", 2, [128, 512], F32, psum=True)
            pO = self.ps(p, "wpO", [128, 512], F32)
            pZ = self.ps(p, "wpZ", [128, 512], F32)
            pM = Ring(nc, p, "wpM", 2, [128, 512], F32, psum=True)
            pTr = Ring(nc, p, "wpT", 3, [128, 4, 128], BF16)
            zr = Ring(nc, p, "wz", 2, [64, 4, 128], F32)
            oTr = Ring(nc, p, "woT", 2, [64, 16, 128], BF16)
            for n in range(32):
                iq = n + 2
                qt, qk = QTr.next()
                S.dma(qt[:], self.qT1[:, :, n * 128:(n + 1) * 128].rearrange("c p n -> p c n"), writes=[qk])
                oT, oTk = oTr.next()
                chunks = [(0, None), (1, None)]
                if iq - 1 >= 2:
                    chunks.append((iq - 1, self.Mbb))
                chunks.append((iq, None))
                if iq + 1 <= 34:
                    chunks.append((iq + 1, self.Mfb))
                last = len(chunks) - 1
                for kvh in range(2):
                    for hh in range(2):
                        sl = slice(hh * 64, (hh + 1) * 64)
                        slot0 = (kvh * 2 + hh) * 4
                        for idx, (kt, mask) in enumerate(chunks):
                            ps, psk = pS.next()
                            S.mm(ps[:], KT[sl, kvh, kt * 128:(kt + 1) * 128], qt[sl, kvh * 4:(kvh + 1) * 4, :], True, True, reads=['wKT', qk], writes=[psk])
                            pT, pTk = pTr.next()
                            S.act(pT[:].rearrange("p h n -> p (h n)"), ps[:], AF.Exp, reads=[psk], writes=[pTk], scale=0.125)
                            if mask is not None:
                                S.tt('pool', pT[:], pT[:], mask[:].unsqueeze(1).to_broadcast([128, 4, 128]), ALU.mult, reads=[pTk], writes=[pTk])
                            pTf = pT[:].rearrange("p h n -> p (h n)")
                            S.mm(pO[0:64, :], V[:, kt, kvh * 64:(kvh + 1) * 64], pTf, idx == 0, idx == last, reads=[pTk, 'wV'], writes=['wpO'])
                            S.mm(pZ[0:64, :], self.onesb[:, 0:64], pTf, idx == 0, idx == last, reads=[pTk, 'onesb'], writes=['wpZ'])
                        z, zk = zr.next()
                        es = ES[0:64, kvh * 8 + hh:kvh * 8 + 8:2].unsqueeze(2).to_broadcast([64, 4, 128])
                        S.tt('dve', z[:], pZ[0:64, :].rearrange("p (h n) -> p h n", h=4), es, ALU.add, reads=['wpZ', 'wES'], writes=[zk, 'wpZ'])
                        S.op('dve', lambda E: E.reciprocal(z[:], z[:]), reads=[zk], writes=[zk])
                        S.tt('dve', oT[:, slot0:slot0 + 4, :], pO[0:64, :].rearrange("p (h n) -> p h n", h=4), z[:], ALU.mult,
                             reads=['wpO', zk], writes=[(oTk, slot0), 'wpO'])
                mps = []
                for nn in range(2):
                    pm, pmk = pM.next()
                    for slot in range(16):
                        kvh, hh, c = slot // 8, (slot // 4) % 2, slot % 4
                        qh = kvh * 8 + 2 * c + hh
                        S.mm(pm[:], oT[:, slot, :], wo[:, qh, nn * 512:(nn + 1) * 512], slot == 0, slot == 15,
                             reads=[(oTk, (slot // 4) * 4)], writes=[pmk])
                    mps.append((pm, pmk))
                self.post_mixer(R, n, n * 128, 0, MT, mps, self.h2[iq * 128:(iq + 1) * 128, :], self.h3, self.fT1, self.gT1, self.fTok1, self.gTok1)
            S.barrier()

    def moe2(self, l, G2, fTd, gTd, fTokd, gTokd, h1d, ntiles, per_pass, ctx_tiles, sink):
        S, nc = self.S, self.nc
        NEX = int(os.environ.get('NEX', 32))
        FORCE = os.environ.get('MOE_FORCE')
        I32 = mybir.dt.int32
        with ExitStack() as p:
            TP = per_pass * 128
            NSUB = (per_pass + 3) // 4
            fT = self.sb(p, "mfT", [128, 8, TP], BF16)
            fTok = self.sb(p, "mfTok", [128, per_pass, 1024], BF16)
            gT = self.sb(p, "mgT", [32, TP], F32)
            gtok = self.sb(p, "mgtok", [128, per_pass, 32], F32)
            mask = self.sb(p, "mmask", [128, per_pass, 32], F32)
            pos1 = self.sb(p, "mpos1", [128, per_pass, 32], F32)
            flagf = self.sb(p, "mflagf", [128, NSUB * 32], F32)
            flagi = self.sb(p, "mflagi", [128, NSUB * 32], I32)
            tris = self.sb(p, "mtris", [128, 128], F32)
            onesf = self.sb(p, "monesf", [128, 128], F32)
            S.tt('dve', tris[:], self.Mf, self.idf, ALU.subtract, reads=['cf'], writes=['mtris'])
            S.memset('dve', onesf[:], 1.0, writes=['monesf'])
            yacc = self.sb(p, "myacc", [128, per_pass, 1024], F32)
            w1r = Ring(nc, p, "mw1", 2, [128, 8, 2048], BF16)
            w2r = Ring(nc, p, "mw2", 2, [128, 8, 1024], BF16)
            b1r = Ring(nc, p, "mb1", 2, [128, 24], F32)
            selr = Ring(nc, p, "msel", 2, [32, 128], F32)
            gB = self.sb(p, "mgB", [128, 512], F32)
            gcr = Ring(nc, p, "mgc", 2, [128, 512], F32)
            sgr = Ring(nc, p, "msg", 2, [128, 512], F32)
            l1r = Ring(nc, p, "ml1", 2, [128, 512], F32)
            ac = self.sb(p, "mac", [128, 8, 512], BF16)
            hxv = ac[:].rearrange("p k n -> p (k n)").bitcast(F32)[:, 0:1024]
            b2s = ac[0:32].rearrange("p k n -> p (k n)").bitcast(F32)[:, 1024:2048]
            Dt2 = [self.sb(p, f"mD{i}", [128, 4, 128], BF16) for i in range(2)]
            Dg = self.sb(p, "mDg", [128, 4, 128], BF16)
            DgT2 = [self.sb(p, f"mDgT{i}", [128, 4, 128], BF16) for i in range(2)]
            XcT2 = [self.sb(p, f"mXcT{i}", [128, 8, 128], BF16) for i in range(2)]
            Ycb = self.sb(p, "mYcb", [128, 1024], BF16)
            pH = [self.ps(p, f"mpH{i}", [128, 512], F32) for i in range(4)]
            pY = [self.ps(p, f"mpY{i}", [128, 512], F32) for i in range(2)]
            pG = self.ps(p, "mpG", [128, 512], F32)
            pT8 = self.ps(p, "mpT8", [128, 4, 128], BF16)
            w1v = lambda e: self.w1[l, e].rearrange("(k p) n -> p k n", p=128)
            w2v = lambda e: self.w2[l, e].rearrange("(k p) n -> p k n", p=128)

            def load_expert(e):
                w1b, w1k = w1r.next()
                w2b, w2k = w2r.next()
                for k in range(8):
                    S.dma(w1b[:, k, :], w1v(e)[:, k, :], writes=[(w1k, k)], q='pool')
                for k in range(0, 8, 2):
                    S.dma(w2b[:, k:k + 2, :], w2v(e)[:, k:k + 2, :], writes=[(w2k, k)], q='pool')
                b1, b1k = b1r.next()
                S.dma(b1[:, 0:16], self.b1[l, e], writes=[b1k])
                S.ts('dve', b1[:, 16:24], b1[:, 8:16], 1.0, None, ALU.add, reads=[b1k], writes=[b1k])
                sl, slk = selr.next()
                S.dma(sl[:], self.sel[:, e * 128:(e + 1) * 128], writes=[slk])
                keys = [(w1k, k) for k in range(8)] + [(w2k, k) for k in range(0, 8, 2)] + [b1k, slk]
                return (w1b, w2b, b1, sl, keys)

            class Rot:
                def __init__(self, items):
                    self.items, self.i = items, 0

                def next(self):
                    j = self.i % len(self.items)
                    self.i += 1
                    return self.items[j], ('rot', id(self.items), j)

            def chain_head(Q, hg, hgk, hl, hlk, b1, j, N):
                gc, gck = gcr.next()
                Q.ts('dve', gc[:, 0:N], hg[:, 0:N], b1[:, j:j + 1], 7.0, ALU.add, ALU.min, reads=[hgk], writes=[gck, hgk])
                sg, sgk = sgr.next()
                Q.act(sg[:, 0:N], gc[:, 0:N], AF.Sigmoid, reads=[gck], writes=[sgk], scale=1.702)
                l1, l1k = l1r.next()
                Q.ts('dve', l1[:, 0:N], hl[:, 0:N], b1[:, 16 + j:17 + j], 8.0, ALU.add, ALU.min, reads=[hlk], writes=[l1k, hlk])
                Q.tt('pool', sg[:, 0:N], gc[:, 0:N], sg[:, 0:N], ALU.mult, reads=[gck, sgk], writes=[sgk])
                return (sg, sgk, l1, l1k)

            def chain_tail(Q, st, N, out_ap, outk, gBap):
                sg, sgk, l1, l1k = st
                if gBap is None:
                    Q.stt('dve', out_ap, l1[:, 0:N], -6.0, sg[:, 0:N], ALU.max, ALU.mult, reads=[l1k, sgk], writes=[outk])
                else:
                    Q.stt('dve', l1[:, 0:N], l1[:, 0:N], -6.0, sg[:, 0:N], ALU.max, ALU.mult, reads=[l1k, sgk], writes=[l1k])
                    Q.tt('pool', out_ap, l1[:, 0:N], gBap, ALU.mult, reads=[l1k, 'gB'], writes=[outk])

            def dense_unit(Q, e, W, c0, N, slot, first, nxt_unit):
                w1b, w2b, b1, sl, _ = W
                pHr = Rot(pH)
                pYr = Rot(pY)
                Q.mm(pG[:, 0:N], sl[0:32, :], gT[0:32, c0:c0 + N], True, True, writes=['pG'])
                Q.copy('act', gB[:, 0:N], pG[:, 0:N], reads=['pG'], writes=['gB', 'pG'])
                prev = None
                for j in range(8):
                    hg, hgk = pHr.next()
                    for k in range(8):
                        Q.mm(hg[:, 0:N], w1b[:, k, j * 128:(j + 1) * 128], fT[:, k, c0:c0 + N], k == 0, k == 7, writes=[hgk])
                    hl, hlk = pHr.next()
                    for k in range(8):
                        Q.mm(hl[:, 0:N], w1b[:, k, 1024 + j * 128:1024 + (j + 1) * 128], fT[:, k, c0:c0 + N], k == 0, k == 7, writes=[hlk])
                    st = chain_head(Q, hg, hgk, hl, hlk, b1, j, N)
                    if prev is not None:
                        chain_tail(Q, prev[0], N, ac[:, prev[1], 0:N], ('ac', prev[1]), gB[:, 0:N])
                    prev = (st, j)
                chain_tail(Q, prev[0], N, ac[:, prev[1], 0:N], ('ac', prev[1]), gB[:, 0:N])
                if nxt_unit is not None:
                    prelude(Q, pHr, nxt_unit[0], nxt_unit[1], nxt_unit[2], 1 - slot)
                for s_ in range(N // 128):
                    ti = (c0 // 128) + s_
                    for n in range(2):
                        py, pyk = pYr.next()
                        for j in range(8):
                            Q.mm(py[:], ac[:, j, s_ * 128:(s_ + 1) * 128], w2b[:, j, n * 512:(n + 1) * 512], j == 0, j == 7,
                                 reads=[('ac', j)], writes=[pyk])
                        ysl = yacc[:, ti, n * 512:(n + 1) * 512]
                        Q.tt('dve', ysl, py[:], ysl, ALU.add, reads=[pyk], writes=[('y', ti, n), pyk])

            def prelude(Q, pHr, e, c0, N, slot):
                Dt, DgT, XcT = Dt2[slot], DgT2[slot], XcT2[slot]
                tl = list(range(c0 // 128, (c0 + N) // 128))
                nr = len(tl)
                for r, t in enumerate(tl):
                    Q.ts('dve', Dt[:, r, :], self.IO1, pos1[:, t, e:e + 1], mask[:, t, e:e + 1], ALU.is_equal, ALU.mult, writes=[('D', slot, r)])
                    Q.ts('dve', Dg[:, r, :], self.IO1, pos1[:, t, e:e + 1], gtok[:, t, e:e + 1], ALU.is_equal, ALU.mult, writes=[('Dg', r)])
                    Q.tr(pT8[:, r, :], Dg[:, r, :], self.idb[:], reads=[('Dg', r)], writes=['pT8'])
                Q.copy('dve', DgT[:, 0:nr, :], pT8[:, 0:nr, :], reads=['pT8'], writes=[('DgT', slot), 'pT8'])
                for half in range(2):
                    ps, psk = pHr.next()
                    for kk in range(4):
                        k = half * 4 + kk
                        for r, t in enumerate(tl):
                            Q.mm(ps[:, kk * 128:(kk + 1) * 128], fTok[:, t, k * 128:(k + 1) * 128], Dt[:, r, :], r == 0, r == nr - 1,
                                 reads=[('D', slot, r)], writes=[psk])
                    Q.copy('dve', XcT[:, half * 4:(half + 1) * 4, :].rearrange("p k n -> p (k n)"), ps[:],
                           reads=[psk], writes=[('Xc', slot, half), psk])

            def sparse_unit(Q, e, W, c0, N, slot, first, nxt_unit):
                w1b, w2b, b1, sl, _ = W
                pHr = Rot(pH)
                tl = list(range(c0 // 128, (c0 + N) // 128))
                if first:
                    prelude(Q, pHr, e, c0, N, slot)
                DgT, XcT = DgT2[slot], XcT2[slot]
                prev = None
                for j in range(8):
                    hg, hgk = pHr.next()
                    for k in range(8):
                        Q.mm(hg[:, 0:128], w1b[:, k, j * 128:(j + 1) * 128], XcT[:, k, :], k == 0, k == 7, reads=[('Xc', slot, k // 4)], writes=[hgk])
                    hl, hlk = pHr.next()
                    for k in range(8):
                        Q.mm(hl[:, 0:128], w1b[:, k, 1024 + j * 128:1024 + (j + 1) * 128], XcT[:, k, :], k == 0, k == 7, reads=[('Xc', slot, k // 4)], writes=[hlk])
                    st = chain_head(Q, hg, hgk, hl, hlk, b1, j, 128)
                    if prev is not None:
                        chain_tail(Q, prev[0], 128, ac[:, prev[1], 0:128], ('ac', prev[1]), None)
                    prev = (st, j)
                chain_tail(Q, prev[0], 128, ac[:, prev[1], 0:128], ('ac', prev[1]), None)
                if nxt_unit is not None:
                    prelude(Q, pHr, nxt_unit[0], nxt_unit[1], nxt_unit[2], 1 - slot)
                for n in range(2):
                    for j in range(8):
                        Q.mm(pY[n][:], ac[:, j, 0:128], w2b[:, j, n * 512:(n + 1) * 512], j == 0, j == 7, reads=[('ac', j)], writes=[('pY', n)])
                for n in range(2):
                    Q.copy('dve', Ycb[:, n * 512:(n + 1) * 512], pY[n][:], reads=[('pY', n)], writes=[('Yc', n), ('pY', n)])
                    for r, t in enumerate(tl):
                        ps, psk = pHr.next()
                        Q.mm(ps[:], DgT[:, r, :], Ycb[:, n * 512:(n + 1) * 512], True, True, reads=[('DgT', slot), ('Yc', n)], writes=[psk])
                        ysl = yacc[:, t, n * 512:(n + 1) * 512]
                        Q.tt('dve', ysl, ps[:], ysl, ALU.add, reads=[psk], writes=[('y', t, n), psk])

            t0 = 0
            while t0 < ntiles:
                nt = min(per_pass, ntiles - t0)
                T = nt * 128
                c_base = t0 * 128
                S.dma(fT[:, :, 0:T], fTd[:, :, c_base:c_base + T].rearrange("k p n -> p k n"), writes=['mfT'])
                S.dma(gT[:, 0:T], gTd[:, c_base:c_base + T], writes=['mgT'])
                S.dma(b2s, self.b2[l], writes=['mb2'])
                S.dma(fTok[:, 0:nt, :], fTokd[c_base:c_base + T, :].rearrange("(t p) d -> p t d", p=128), writes=['mfTok'])
                S.dma(gtok[:, 0:nt, :], gTokd[c_base:c_base + T, :].rearrange("(t p) e -> p t e", p=128), writes=['mgtok'])
                S.ts('dve', mask[:, 0:nt, :], gtok[:, 0:nt, :], 0.0, None, ALU.is_gt, reads=['mgtok'], writes=['mmask'])
                subs = [(c, min(512, T - c)) for c in range(0, T, 512)]
                for si, (c0, N) in enumerate(subs):
                    tl = list(range(c0 // 128, (c0 + N) // 128))
                    for r, t in enumerate(tl):
                        S.mm(pG[:, 0:32], tris[:], mask[:, t, :], True, r == 0, reads=['mmask', 'mtris'], writes=['pG'])
                        for r2 in range(r):
                            S.mm(pG[:, 0:32], onesf[:], mask[:, tl[r2], :], False, r2 == r - 1, reads=['mmask', 'monesf'], writes=['pG'])
                        S.ts('dve', pos1[:, t, :], pG[:, 0:32], 1.0, None, ALU.add, reads=['pG'], writes=[('pos1', t), 'pG'])
                    for r, t in enumerate(tl):
                        S.mm(pG[:, 0:32], onesf[:], mask[:, t, :], r == 0, r == len(tl) - 1, reads=['mmask', 'monesf'], writes=['pG'])
                    S.ts('dve', flagf[:, si * 32:(si + 1) * 32], pG[:, 0:32], 128.0, None, ALU.is_gt, reads=['pG'], writes=['mflagf', 'pG'])
                S.copy('dve', flagi[:], flagf[:], reads=['mflagf'], writes=['mflagi'])
                for s_ in range(nt):
                    for n in range(2):
                        ps = pY[n]
                        S.mm(ps[:], gT[0:32, s_ * 128:(s_ + 1) * 128], b2s[:, n * 512:(n + 1) * 512], True, True, reads=['mgT', 'mb2'], writes=[('pY', n)])
                        S.copy('act', yacc[:, s_, n * 512:(n + 1) * 512], ps[:], reads=[('pY', n)], writes=[('y', s_, n), ('pY', n)])
                nxt = load_expert(0)
                S.barrier()
                for e in range(NEX):
                    W = nxt
                    S.sync_keys(W[4])
                    if e + 1 < NEX:
                        nxt = load_expert(e + 1)
                    for si, (c0, N) in enumerate(subs):
                        slot = getattr(S, 'units', 0) % 2
                        first = (e == 0 and si == 0)
                        if si + 1 < len(subs):
                            nu = (e, subs[si + 1][0], subs[si + 1][1])
                        elif e + 1 < NEX:
                            nu = (e + 1, subs[0][0], subs[0][1])
                        else:
                            nu = None
                        if FORCE:
                            Q = S.unit_begin()
                            (sparse_unit if FORCE == 'sparse' else dense_unit)(Q, e, W, c0, N, slot, first, nu)
                            S.unit_end(Q)
                        else:
                            if not hasattr(self, 'flag_regs'):
                                self.flag_regs = nc.alloc_registers("moeflag", engines=mybir.ALL_ENGINES)
                            nc.regs_load(self.flag_regs, flagi[0:1, si * 32 + e:si * 32 + e + 1])
                            Q = S.unit_begin()
                            with nc.If_eq(self.flag_regs, 0):
                                sparse_unit(Q, e, W, c0, N, slot, first, nu)
                                S.unit_end(Q)
                            Q2 = Sched(nc, S.stack, parent=S, setid=Q.setid)
                            with nc.Else():
                                dense_unit(Q2, e, W, c0, N, slot, first, nu)
                                S.unit_end(Q2)
                        S.unit_done()
                hs = S.sem(('HS',))
                for e_ in ENG:
                    S.E[e_].wait_ge(hs, S.units)
                for s_ in range(nt):
                    gi = t0 + s_
                    t = 1 if gi in ctx_tiles else 0
                    S.dma(hxv, h1d[gi * 128:(gi + 1) * 128, :], writes=['mhx'])
                    S.tt('dve', yacc[:, s_, :], yacc[:, s_, :], G2[:, t, :], ALU.mult, writes=[('yy', s_)])
                    S.tt('pool', hxv, yacc[:, s_, :], hxv, ALU.add, reads=[('yy', s_), 'mhx'], writes=['mhx'])
                    sink(gi, hxv, 'mhx', yacc[:, s_, :], ('yy', s_))
                S.barrier()
                t0 += nt
            S.barrier()

    def build(self):
        with self.top:
            self._build()
            self.S.finish()
        return self.nc

    def _build(self):
        S, nc = self.S, self.nc
        self.consts()
        G2 = self.sb(self.top, "G2", [128, 2, 1024], F32)
        self.h2 = self.scr("h2", [NQ, 1024], F32)
        with ExitStack() as L0:
            MT = [self.sb(L0, f"MT{t}", [128, 6144], F32) for t in range(2)]
            self.mod_phase(0, MT)
            if 'MT' in self.dbg:
                o = nc.dram_tensor("MTd", [2, 128, 6144], F32, kind="ExternalOutput").ap()
                for t in range(2):
                    for g in range(12):
                        S.dma(o[t][:, g * 512:(g + 1) * 512], MT[t][:, g * 512:(g + 1) * 512])
            if self.stop == 'mod':
                return
            self.ret_lg(L0)
            self.l0_A(MT)
            if self.stop == 'A':
                return
            self.l0_R()
            if self.stop == 'R':
                return
            self.l0_D()
            if self.stop == 'D':
                return
            self.l0_F(MT)
            for t in range(2):
                S.copy('dve', G2[:, t, :], MT[t][:, 5120:6144], writes=['G2'])
            S.barrier()
            if self.stop == 'F':
                return

        def sink0(gi, h2, h2k, scr=None, scrk=None):
            S.dma(self.h2[gi * 128:(gi + 1) * 128, :], h2, reads=[h2k], q='sp')
        self.moe2(0, G2, self.fT0, self.gT0, self.fTok0, self.gTok0, self.h1, NT_Q, 7, (0, 1), sink0)
        if self.stop == 'M0':
            return
        fnt = G2[:, 1:2, :].rearrange("p o d -> p (o d)")
        S.barrier()
        S.dma(fnt, self.fnw.partition_broadcast(128), writes=['fnt'])
        frs = Ring(nc, self.top, "frs", 3, [128, 4], F32)
        with ExitStack() as L1:
            MT = [self.sb(L1, f"MU{t}", [128, 6144], F32) for t in range(2)]
            self.mod_phase(1, MT)
            self.l1_A(MT)
            if self.stop == 'A1':
                return
            self.l1_W(MT)
            S.copy('dve', G2[:, 0, :], MT[0][:, 5120:6144], writes=['G2'])
            S.barrier()
            if self.stop == 'W1':
                return

        def sink1(gi, h2, h2k, scr=None, scrk=None):
            rs, rk = frs.next()
            S.act(scr, h2, AF.Square, reads=[h2k], writes=[scrk, rk], accum_out=rs[:, 0:1])
            S.ts('dve', rs[:, 1:2], rs[:, 0:1], 1.0 / 1024, 1e-6, ALU.mult, ALU.add, reads=[rk], writes=[rk])
            S.op('act', lambda E: E.sqrt(rs[:, 2:3], rs[:, 1:2]), reads=[rk], writes=[rk])
            S.op('dve', lambda E: E.reciprocal(rs[:, 3:4], rs[:, 2:3]), reads=[rk], writes=[rk])
            S.stt('dve', scr, h2, rs[:, 3:4], fnt, ALU.mult, ALU.mult, reads=[h2k, rk, 'fnt'], writes=[scrk])
            S.dma(self.out[gi * 128:(gi + 1) * 128, :], scr, reads=[scrk], q='sp')
        self.moe2(1, G2, self.fT1, self.gT1, self.fTok1, self.gTok1, self.h3, 32, 7, (), sink1)


def host_prep(inp, cid):
    b, half = cid // 2, cid % 2
    x, ctx = inp['x'][b], inp['ctx'][b]
    pos = np.arange(SEQ)
    if half == 1:
        x, ctx, pos = x[::-1], ctx[::-1], pos[::-1]
    m = {}
    m['xl'] = np.ascontiguousarray(np.concatenate([ctx, x], axis=0), dtype=np.float32)
    inv = (10000.0 ** (-np.arange(16, dtype=np.float32) / 16)).astype(np.float32)
    row = (pos // 64).astype(np.float32); col = (pos % 64).astype(np.float32)
    ar = row[:, None] * inv[None, :]; ac = col[:, None] * inv[None, :]
    rp = np.concatenate([np.cos(ar), np.cos(ac), np.sin(ar), np.sin(ac)], axis=1).astype(np.float32)
    rp0 = np.zeros((CTXL, 64), np.float32); rp0[:, 0:32] = 1.0
    m['rope'] = np.ascontiguousarray(np.concatenate([rp0, rp], axis=0))
    cc = np.stack([inp['c'][b], inp['c_ctx']], axis=-1)
    m['cc'] = np.ascontiguousarray(cc.reshape(8, 128, 2).transpose(1, 0, 2).reshape(128, 16))
    j = np.arange(128)[:, None].astype(np.float32); i = np.arange(128)[None, :].astype(np.float32)
    cst = np.zeros((128, 898), np.float32)
    cst[:, 0:128] = np.eye(128)
    cst[:, 128:256] = np.maximum(i - j, 0); cst[:, 256:384] = (i >= j)
    cst[:, 384:512] = np.maximum(j - i, 0); cst[:, 512:640] = (j >= i)
    cst[:, 640:768] = i + 1 + 0 * j; cst[:, 768:896] = 128 - i + 0 * j
    cst[:, 896] = 127 - j[:, 0]; cst[:, 897] = j[:, 0]
    m['cst'] = cst
    sel = np.zeros((32, 32, 128), np.float32)
    for e in range(32):
        sel[e, e, :] = 1.0
    m['sel'] = sel.reshape(32, 4096)
    for k in ('mod_w', 'mod_b', 'norm1_w', 'norm2_w', 'router_w', 'router_b', 'moe_w1', 'moe_w2', 'moe_b2'):
        m[k] = inp[k]
    m['final_norm_w'] = inp['final_norm_w'].reshape(1, 1024)
    m['even_w_in'] = inp['even_w_in'][0]; m['even_w_out'] = inp['even_w_out'][0]
    m['diff_lam'] = inp['diff_lam'].reshape(1, 256); m['diff_norm_w'] = inp['diff_norm_w'].reshape(128, 1)
    rdl = inp['ret_decay_logit'][0]
    if half == 1:
        rdl = rdl[::-1]
    m['ret_decay_logit'] = np.ascontiguousarray(rdl).reshape(1, 8)
    m['ret_norm_w'] = inp['ret_norm_w'].reshape(128, 1)
    m['odd_w_qkv'] = inp['odd_w_qkv'][0]; m['odd_w_out'] = inp['odd_w_out'][0]; m['odd_sinks'] = inp['odd_sinks'].reshape(1, 16)
    m['moe_b1'] = np.ascontiguousarray(inp['moe_b1'].reshape(2, 32, 16, 128).transpose(0, 1, 3, 2))
    return {k: np.ascontiguousarray(v, dtype=np.float32) for k, v in m.items()}


def kernel(**inputs):
    inp = {k: np.asarray(v) for k, v in inputs.items()}
    nc = K().build()
    in_maps = [host_prep(inp, cid) for cid in range(8)]
    res = run_bass_kernel_spmd(nc, in_maps, core_ids=list(range(8)))
    out = np.zeros((4, SEQ, 1024), np.float32)
    for cid in range(8):
        b, half = cid // 2, cid % 2
        o = res.results[cid]["out"]
        if half == 0:
            out[b, 0:4096] = o
        else:
            out[b, 4096:] = o[::-1]
    return out
```

```python
import os
import numpy as np
from contextlib import ExitStack
import concourse.bass as bass
import concourse.mybir as mybir
from concourse.bass_utils import run_bass_kernel_spmd

F32 = mybir.dt.float32
BF16 = mybir.dt.bfloat16
AF = mybir.ActivationFunctionType
ALU = mybir.AluOpType
AX = mybir.AxisListType
ENG = ('pe', 'act', 'dve', 'pool', 'sp')

SEQ = 8192
CTXL = 256
NE = SEQ + CTXL
NT_ALL = NE // 128
NT_Q = 35
NQ = NT_Q * 128
NEXP = 32


class Sched:
    ROLL = 30000
    NDMA = 12

    def __init__(self, nc, stack, parent=None, setid=0):
        self.parent, self.setid = parent, setid
        if parent is not None:
            self.NDMA = 4
        self.nc, self.stack = nc, stack
        self.E = {'pe': nc.tensor, 'act': nc.scalar, 'dve': nc.vector, 'pool': nc.gpsimd, 'sp': nc.sync}
        self.cnt = {e: 0 for e in ENG}
        self.dcnt = {e: 0 for e in ENG}
        self.sems = {}
        self.waited = {e: {} for e in ENG}
        self.bufs = {}
        self.last = {}
        self.nwaits = 0

    def sem(self, key):
        if self.parent is not None:
            return self.parent.sem(('c', self.setid) + tuple(key))
        s = self.sems.get(key)
        if s is None:
            s = self.stack.enter_context(self.nc.semaphore("s_" + "_".join(map(str, key))))
            self.sems[key] = s
        return s

    def _wait(self, e, k, v):
        wd = self.waited[e]
        if wd.get(k, 0) >= v:
            return
        wd[k] = v
        self.E[e].wait_ge(self.sem(k), v)
        self.nwaits += 1

    def op(self, e, fn, reads=(), writes=(), dma=False):
        need = {}
        for r in reads:
            b = self.bufs.get(r)
            if b:
                for k, v in b[0].items():
                    if need.get(k, 0) < v:
                        need[k] = v
        for w in writes:
            b = self.bufs.get(w)
            if b:
                for d in b:
                    for k, v in d.items():
                        if need.get(k, 0) < v:
                            need[k] = v
        if dma:
            m = self.dcnt[e]
            self.dcnt[e] += 1
            sk = (e, 'd', m % self.NDMA)
            val = 16 * (m // self.NDMA + 1)
            if m >= self.NDMA:
                need[sk] = max(need.get(sk, 0), val - 16)
            inc = 16
        else:
            n = self.cnt[e]
            self.cnt[e] += 1
            sk = (e, 'c', n // self.ROLL)
            val = n % self.ROLL + 1
            inc = 1
        for k, v in need.items():
            if e == 'pe' and k[0] == 'pe' and k[1] == 'c':
                continue
            self._wait(e, k, v)
        fn(self.E[e]).then_inc(self.sem(sk), inc)
        self.last[sk] = val
        for r in reads:
            b = self.bufs.setdefault(r, [{}, {}])
            if b[1].get(sk, 0) < val:
                b[1][sk] = val
        for w in writes:
            self.bufs[w] = [{sk: val}, {}]

    def barrier(self):
        for e in ENG:
            for k, v in self.last.items():
                if k[0] == e and k[1] == 'c':
                    continue
                self._wait(e, k, v)
        self.bufs = {}

    def finish(self):
        for k, v in self.last.items():
            self._wait('sp', k, v)

    def sync_keys(self, keys):
        for key in keys:
            b = self.bufs.get(key)
            if b:
                for e in ENG:
                    for k, v in b[0].items():
                        if e == 'pe' and k[0] == 'pe' and k[1] == 'c':
                            continue
                        self._wait(e, k, v)

    def unit_begin(self):
        u = getattr(self, 'units', 0)
        if u > 0:
            hs = self.sem(('HS',))
            for e in ENG:
                self.E[e].wait_ge(hs, u)
        return Sched(self.nc, self.stack, parent=self, setid=u % 2)

    def unit_end(self, child):
        child.barrier()
        other = 1 - child.setid
        sp = self.E['sp']
        for key, sm in list(self.sems.items()):
            if len(key) > 1 and key[0] == 'c' and key[1] == other:
                sp.sem_clear(sm)
        sp.sem_inc(self.sem(('HS',)), 1)

    def unit_done(self):
        self.units = getattr(self, 'units', 0) + 1

    def dma(self, out, in_, reads=(), writes=(), q='sp', **kw):
        self.op(q, lambda E: E.dma_start(out=out, in_=in_, **kw), reads, writes, dma=True)

    def mm(self, out, lhsT, rhs, start, stop, reads=(), writes=()):
        self.op('pe', lambda E: E.matmul(out, lhsT, rhs, start=start, stop=stop), reads, writes)

    def tr(self, out, in_, ident, reads=(), writes=()):
        self.op('pe', lambda E: E.transpose(out, in_, ident), reads, writes)

    def act(self, out, in_, func, reads=(), writes=(), **kw):
        self.op('act', lambda E: E.activation(out, in_, func, **kw), reads, writes)

    def ts(self, e, out, in0, s1, s2, op0, op1=None, reads=(), writes=(), **kw):
        if op1 is None:
            self.op(e, lambda E: E.tensor_scalar(out, in0, s1, None, op0, **kw), reads, writes)
        else:
            self.op(e, lambda E: E.tensor_scalar(out, in0, s1, s2, op0, op1, **kw), reads, writes)

    def tt(self, e, out, in0, in1, op, reads=(), writes=()):
        self.op(e, lambda E: E.tensor_tensor(out, in0, in1, op), reads, writes)

    def stt(self, e, out, in0, scalar, in1, op0, op1, reads=(), writes=()):
        self.op(e, lambda E: E.scalar_tensor_tensor(out, in0, scalar, in1, op0, op1), reads, writes)

    def copy(self, e, out, in_, reads=(), writes=()):
        if e == 'act':
            self.op(e, lambda E: E.copy(out, in_), reads, writes)
        else:
            self.op(e, lambda E: E.tensor_copy(out, in_), reads, writes)

    def memset(self, e, ap, val, writes=()):
        self.op(e, lambda E: E.memset(ap, val), (), writes)


_UNIQ = [0]


class Ring:
    def __init__(self, nc, st, name, n, shape, dtype, psum=False):
        mk = nc.psum_tensor if psum else nc.sbuf_tensor
        _UNIQ[0] += 1
        name = f"{name}_{_UNIQ[0]}_"
        self.t = [st.enter_context(mk(f"{name}{i}", shape, dtype)) for i in range(n)]
        self.name, self.n, self.i = name, n, 0

    def next(self):
        j = self.i % self.n
        self.i += 1
        return self.t[j], (self.name, j)


class K:
    def __init__(self, dbg=(), stop=None):
        self.dbg, self.stop = set(dbg), stop
        self.nc = nc = bass.Bass("TRN2", target_bir_lowering=False)
        self.top = ExitStack()
        self.S = Sched(nc, self.top)
        i = self.din
        self.xl = i("xl", [NE, 1024]); self.rope = i("rope", [NE, 64]); self.cc = i("cc", [128, 16])
        self.cst = i("cst", [128, 898]); self.sel = i("sel", [32, 4096])
        self.mod_w = i("mod_w", [2, 1024, 6144]); self.mod_b = i("mod_b", [2, 6144])
        self.n1w = i("norm1_w", [2, 1024]); self.n2w = i("norm2_w", [2, 1024]); self.fnw = i("final_norm_w", [1, 1024])
        self.w_in = i("even_w_in", [1024, 3072]); self.w_out0 = i("even_w_out", [1024, 1024])
        self.dlam = i("diff_lam", [1, 256]); self.dnw = i("diff_norm_w", [128, 1]); self.rdl = i("ret_decay_logit", [1, 8])
        self.rnw = i("ret_norm_w", [128, 1])
        self.w_qkv = i("odd_w_qkv", [1024, 1280]); self.w_out1 = i("odd_w_out", [1024, 1024]); self.sinks = i("odd_sinks", [1, 16])
        self.rw = i("router_w", [2, 1024, 32]); self.rb = i("router_b", [2, 32])
        ne = 1 if stop in ('mod', 'A', 'R', 'D', 'F') else 32
        self.w1 = i("moe_w1", [2, ne, 1024, 2048]); self.b1 = i("moe_b1", [2, 32, 128, 16])
        self.w2 = i("moe_w2", [2, ne, 1024, 1024]); self.b2 = i("moe_b2", [2, 32, 1024])
        self.out = nc.dram_tensor("out", [4096, 1024], F32, kind="ExternalOutput").ap()

    def din(self, name, shape, dt=F32):
        return self.nc.dram_tensor(name, shape, dt, kind="ExternalInput").ap()

    def scr(self, name, shape, dt):
        kind = "ExternalOutput" if name in self.dbg else "Internal"
        return self.nc.dram_tensor(name, shape, dt, kind=kind).ap()

    def sb(self, st, name, shape, dt):
        _UNIQ[0] += 1
        return st.enter_context(self.nc.sbuf_tensor(f"{name}_{_UNIQ[0]}", shape, dt))

    def ps(self, st, name, shape, dt):
        _UNIQ[0] += 1
        return st.enter_context(self.nc.psum_tensor(f"{name}_{_UNIQ[0]}", shape, dt))

    def consts(self):
        S, st = self.S, self.top
        self.cf = self.sb(st, "cf", [128, 898], F32)
        S.dma(self.cf[:], self.cst, writes=['cf'])
        self.idb = self.sb(st, "idb", [128, 128], BF16)
        S.copy('dve', self.idb[:], self.cf[:, 0:128], reads=['cf'], writes=['idb'])
        self.onesb = self.sb(st, "onesb", [128, 128], BF16)
        S.memset('dve', self.onesb[:], 1.0, writes=['onesb'])
        c = self.cf
        self.idf = c[:, 0:128]
        self.Af, self.Mf, self.Ab, self.Mb = c[:, 128:256], c[:, 256:384], c[:, 384:512], c[:, 512:640]
        self.IO1, self.IO2 = c[:, 640:768], c[:, 768:896]
        self.C1, self.C2 = c[:, 896:897], c[:, 897:898]
        self.Mfb = self.sb(st, "Mfb", [128, 128], BF16)
        self.Mbb = self.sb(st, "Mbb", [128, 128], BF16)
        S.copy('dve', self.Mfb[:], self.Mf, reads=['cf'], writes=['Mfb'])
        S.copy('dve', self.Mbb[:], self.Mb, reads=['cf'], writes=['Mbb'])

    def mod_phase(self, l, MT):
        S, nc = self.S, self.nc
        with ExitStack() as p:
            cT = self.sb(p, "cT", [128, 16], F32)
            sc = self.sb(p, "scT", [128, 16], F32)
            cB = self.sb(p, "cB", [128, 2, 8, 128], F32)
            S.dma(cT[:], self.cc, writes=['cT'])
            S.act(sc[:], cT[:], AF.Silu, reads=['cT'], writes=['sc'])
            scv = sc[:].rearrange("p (k t) -> p k t", t=2)
            for t in range(2):
                S.copy('dve', cB[:, t], scv[:, :, t:t + 1].to_broadcast([128, 8, 128]), reads=['sc'], writes=['cB'])
            wr = Ring(nc, p, "mw", 2, [128, 8, 512], F32)
            mbr = Ring(nc, p, "mb", 2, [128, 512], F32)
            pr = Ring(nc, p, "mps", 2, [128, 512], F32, psum=True)
            mw = self.mod_w[l].rearrange("(k p) n -> p k n", p=128)
            for g in range(12):
                w, wk = wr.next()
                S.dma(w[:], mw[:, :, g * 512:(g + 1) * 512], writes=[wk])
                mb, mbk = mbr.next()
                S.dma(mb[:], self.mod_b[l:l + 1, g * 512:(g + 1) * 512].partition_broadcast(128), writes=[mbk])
                for t in range(2):
                    ps, pk = pr.next()
                    for k in range(8):
                        S.mm(ps[:], cB[:, t, k, :], w[:, k, :], k == 0, k == 7, reads=[wk, 'cB'], writes=[pk])
                    S.tt('dve', MT[t][:, g * 512:(g + 1) * 512], ps[:], mb[:], ALU.add, reads=[pk, mbk], writes=[('MT', t, g)])
            nw = self.sb(p, "nw", [128, 2, 1024], F32)
            S.dma(nw[:, 0, :], self.n1w[l:l + 1, :].partition_broadcast(128), writes=['nw0'])
            S.dma(nw[:, 1, :], self.n2w[l:l + 1, :].partition_broadcast(128), writes=['nw1'])
            S.barrier()
            for t in range(2):
                for j, o in enumerate((1024, 4096)):
                    S.stt('dve', MT[t][:, o:o + 1024], MT[t][:, o:o + 1024], 1.0, nw[:, j, :], ALU.add, ALU.mult,
                          writes=[('MTs', t, j)])
            S.barrier()

    def front(self, xt, xk, S1, B1, rs_ring, tmp_ring, ab_ring, pT_ring, aT_ring, tabkeys=()):
        S = self.S
        rs, rk = rs_ring.next()
        tmp, tk = tmp_ring.next()
        S.act(tmp[:], xt, AF.Square, reads=[xk], writes=[tk, rk], accum_out=rs[:, 0:1])
        S.ts('dve', rs[:, 1:2], rs[:, 0:1], 1.0 / 1024, 1e-6, ALU.mult, ALU.add, reads=[rk], writes=[rk])
        S.op('act', lambda E: E.sqrt(rs[:, 2:3], rs[:, 1:2]), reads=[rk], writes=[rk])
        S.op('dve', lambda E: E.reciprocal(rs[:, 3:4], rs[:, 2:3]), reads=[rk], writes=[rk])
        S.stt('dve', tmp[:], xt, rs[:, 3:4], S1, ALU.mult, ALU.mult, reads=[xk, rk] + list(tabkeys), writes=[tk])
        ab, abk = ab_ring.next()
        S.tt('pool', ab[:], tmp[:], B1, ALU.add, reads=[tk] + list(tabkeys), writes=[abk])
        pT, pTk = pT_ring.next()
        for k in range(8):
            S.tr(pT[:, k, :], ab[:, k * 128:(k + 1) * 128], self.idb[:], reads=[abk, 'idb'], writes=[pTk])
        aT, aTk = aT_ring.next()
        S.copy('act', aT[:], pT[:], reads=[pTk], writes=[aTk])
        self.last_ab = (ab, abk)
        return aT, aTk

    def load_w_bf16(self, st, name, src, ncols, stage_ring):
        S = self.S
        w = self.sb(st, name, [128, 8, ncols], BF16)
        sv = src.rearrange("(k p) n -> p k n", p=128)
        for k in range(8):
            for c0 in range(0, ncols, 1024):
                c1 = min(ncols, c0 + 1024)
                stg, sk = stage_ring.next()
                S.dma(stg[:, 0:c1 - c0], sv[:, k, c0:c1], writes=[sk])
                S.copy('pool', w[:, k, c0:c1], stg[:, 0:c1 - c0], reads=[sk], writes=[(name, k, c0)])
        return w

    def l0_A(self, MT):
        S, nc = self.S, self.nc
        d = self.scr
        self.qT0 = d("qT0", [4, 128, NQ], BF16); self.kT0 = d("kT0", [4, 128, NE], BF16); self.v0 = d("v0", [NE, 512], BF16)
        self.rkf = d("rkf", [NE, 256], BF16); self.rkb = d("rkb", [NE, 256], BF16); self.rkT = d("rkT", [2, 128, NE], BF16)
        self.rv0 = d("rv0", [NE, 512], BF16)
        self.rqT = d("rqT", [3, 2, 128, NQ], BF16)
        self.sgT = d("sgT", [4, 128, NQ], BF16)
        with ExitStack() as p:
            stg = Ring(nc, p, "stg", 2, [128, 1024], F32)
            w = self.load_w_bf16(p, "win", self.w_in, 3072, stg)
            lg = self.lg
            KF = self.sb(p, "KF", [128, 2, 4], F32)
            QD = self.sb(p, "QD", [128, 2, 2, 128], F32)
            for dr in range(2):
                for h in range(4):
                    S.act(KF[:, dr, h:h + 1], self.C1 if dr == 0 else self.C2, AF.Exp, reads=['lg', 'cf'], writes=['KF'],
                          scale=lg[:, dr * 4 + h:dr * 4 + h + 1])
                    c, hh = h // 2, h % 2
                    S.act(QD[hh * 64:(hh + 1) * 64, dr, c, :], (self.IO1 if dr == 0 else self.IO2)[hh * 64:(hh + 1) * 64, :], AF.Exp,
                          reads=['lg', 'cf'], writes=['QD'], scale=lg[hh * 64:(hh + 1) * 64, dr * 4 + h:dr * 4 + h + 1])
            xr = Ring(nc, p, "xt", 3, [128, 1024], F32)
            rpr = Ring(nc, p, "rp", 3, [128, 64], F32)
            rsr = Ring(nc, p, "rs", 3, [128, 4], F32)
            tmr = Ring(nc, p, "tmp", 2, [128, 1024], F32)
            abr = Ring(nc, p, "ab", 2, [128, 1024], BF16)
            aTr = Ring(nc, p, "aT", 2, [128, 8, 128], BF16)
            pTr = Ring(nc, p, "pT", 2, [128, 8, 128], BF16, psum=True)
            pPr = Ring(nc, p, "pP", 3, [128, 512], F32, psum=True)
            pFr = Ring(nc, p, "pF", 2, [128, 4, 128], F32, psum=True)
            pQr = Ring(nc, p, "pQ", 1, [128, 4, 128], BF16, psum=True)
            r1 = Ring(nc, p, "r1", 2, [128, 4, 256], F32)
            qrr = Ring(nc, p, "qr", 2, [128, 512], BF16)
            qTs = Ring(nc, p, "qTs", 3, [128, 4, 128], BF16)
            vbr = Ring(nc, p, "vb", 3, [128, 512], BF16)
            kdr = Ring(nc, p, "kd", 3, [128, 2, 256], BF16)
            fTr = Ring(nc, p, "fTs", 3, [128, 4, 128], BF16)
            q3r = Ring(nc, p, "q3", 2, [128, 3, 2, 128], BF16)

            def tokmajor(aT, aTk, c0, n):
                ps, pk = pPr.next()
                for k in range(8):
                    S.mm(ps[:, 0:n], aT[:, k, :], w[:, k, c0:c0 + n], k == 0, k == 7, reads=[aTk], writes=[pk])
                return ps, pk

            def featmajor(aT, aTk, c0, nch):
                ps, pk = pFr.next()
                for j in range(nch):
                    for k in range(8):
                        S.mm(ps[:, j, :], w[:, k, c0 + j * 128:c0 + (j + 1) * 128], aT[:, k, :], k == 0, k == 7, reads=[aTk], writes=[pk])
                return ps, pk

            def rope_T(ps, pk, rp, rpk, dst, col0):
                xv = ps[:].rearrange("p (h r a f) -> p h r a f", h=8, r=2, a=2, f=16)
                a, b = xv[:, :, :, 0, :], xv[:, :, :, 1, :]
                cos = rp[:, 0:32].rearrange("p (r f) -> p r f", r=2).unsqueeze(1).to_broadcast([128, 8, 2, 16])
                sin = rp[:, 32:64].rearrange("p (r f) -> p r f", r=2).unsqueeze(1).to_broadcast([128, 8, 2, 16])
                t, tk = r1.next()
                tv = [t[:, i, :].rearrange("p (h r f) -> p h r f", h=8, r=2) for i in range(4)]
                S.tt('dve', tv[0], a, cos, ALU.mult, reads=[pk, rpk], writes=[(tk, 0)])
                S.tt('dve', tv[1], b, sin, ALU.mult, reads=[pk, rpk], writes=[(tk, 1)])
                S.tt('dve', tv[2], a, sin, ALU.mult, reads=[pk, rpk], writes=[(tk, 2)])
                S.tt('dve', tv[3], b, cos, ALU.mult, reads=[pk, rpk], writes=[(tk, 3)])
                qr, qk = qrr.next()
                qv = qr[:].rearrange("p (h r a f) -> p h r a f", h=8, r=2, a=2, f=16)
                S.tt('pool', qv[:, :, :, 0, :], tv[0], tv[1], ALU.subtract, reads=[(tk, 0), (tk, 1)], writes=[(qk, 0)])
                S.tt('pool', qv[:, :, :, 1, :], tv[2], tv[3], ALU.add, reads=[(tk, 2), (tk, 3)], writes=[(qk, 1)])
                pq, pqk = pQr.next()
                for h in range(4):
                    S.tr(pq[:, h, :], qr[:, h * 128:(h + 1) * 128], self.idb[:], reads=[(qk, 0), (qk, 1)], writes=[pqk])
                qs, qsk = qTs.next()
                S.copy('act', qs[:], pq[:], reads=[pqk], writes=[qsk])
                S.dma(dst[:, :, col0:col0 + 128].rearrange("h p n -> p h n"), qs[:], reads=[qsk], writes=[], q='pool')

            KLIM = int(os.environ.get('KLIM', NT_ALL)); KCUT = int(os.environ.get('KCUT', 99))
            for i in range(min(NT_ALL, KLIM)):
                t = 1 if i < 2 else 0
                needq = i < NT_Q
                r0 = i * 128
                xt, xk = xr.next()
                S.dma(xt[:], self.xl[r0:r0 + 128, :], writes=[xk])
                rp, rpk = rpr.next()
                S.dma(rp[:], self.rope[r0:r0 + 128, :], writes=[rpk])
                aT, aTk = self.front(xt[:], xk, MT[t][:, 1024:2048], MT[t][:, 0:1024], rsr, tmr, abr, pTr, aTr)
                if KCUT < 1: continue
                ps, pk = tokmajor(aT, aTk, 512, 512)
                rope_T(ps, pk, rp, rpk, self.kT0, r0)
                if needq:
                    ps, pk = tokmajor(aT, aTk, 0, 512)
                    rope_T(ps, pk, rp, rpk, self.qT0, r0)
                if KCUT < 2: continue
                ps, pk = tokmajor(aT, aTk, 1024, 512)
                vb, vk = vbr.next()
                S.copy('act', vb[:], ps[:], reads=[pk], writes=[vk])
                S.dma(self.v0[r0:r0 + 128, :], vb[:], reads=[vk], q='pool')
                ps, pk = tokmajor(aT, aTk, 2048, 512)
                vb, vk = vbr.next()
                S.copy('act', vb[:], ps[:], reads=[pk], writes=[vk])
                S.dma(self.rv0[r0:r0 + 128, :], vb[:], reads=[vk], q='pool')
                if KCUT < 3: continue
                ps, pk = tokmajor(aT, aTk, 1792, 256)
                kd, kk = kdr.next()
                for dr in range(2):
                    S.tt('dve', kd[:, dr, :].rearrange("p (h d) -> p h d", h=4), ps[:, 0:256].rearrange("p (h d) -> p h d", h=4),
                         KF[:, dr, :].unsqueeze(2).to_broadcast([128, 4, 64]), ALU.mult, reads=[pk, 'KF'], writes=[(kk, dr)])
                S.dma(self.rkf[r0:r0 + 128, :], kd[:, 0, :], reads=[(kk, 0)], q='pool')
                S.dma(self.rkb[r0:r0 + 128, :], kd[:, 1, :], reads=[(kk, 1)], q='pool')
                if KCUT < 4: continue
                ps, pk = featmajor(aT, aTk, 1792, 2)
                fT, fk = fTr.next()
                S.copy('act', fT[:, 0:2, :], ps[:, 0:2, :], reads=[pk], writes=[fk])
                S.dma(self.rkT[:, :, r0:r0 + 128].rearrange("c p n -> p c n"), fT[:, 0:2, :], reads=[fk], q='pool')
                if KCUT < 5: continue
                if needq:
                    ps, pk = featmajor(aT, aTk, 1536, 2)
                    q3, q3k = q3r.next()
                    S.op('act', lambda E: E.mul(q3[:, 0, :, :], ps[:, 0:2, :], 0.125), reads=[pk], writes=[(q3k, 0), pk])
                    for dr in range(2):
                        S.stt('dve', q3[:, 1 + dr, :, :], ps[:, 0:2, :], 0.125, QD[:, dr, :, :], ALU.mult, ALU.mult,
                              reads=[pk, 'QD'], writes=[(q3k, 1 + dr), pk])
                    for v in range(3):
                        S.dma(self.rqT[v, :, :, r0:r0 + 128].rearrange("c p n -> p c n"), q3[:, v, :, :], reads=[(q3k, v)], q='pool')
                    if KCUT < 6: continue
                    ps, pk = featmajor(aT, aTk, 2560, 4)
                    fT, fk = fTr.next()
                    S.act(fT[:], ps[:], AF.Silu, reads=[pk], writes=[fk])
                    S.dma(self.sgT[:, :, r0:r0 + 128].rearrange("c p n -> p c n"), fT[:], reads=[fk], q='pool')
            S.barrier()


    def ret_lg(self, st):
        S = self.S
        dl = self.sb(st, "dl", [128, 8], F32)
        S.dma(dl[:], self.rdl.partition_broadcast(128), writes=['dl'])
        self.lg = lg = self.sb(st, "lg", [128, 8], F32)
        S.act(lg[:], dl[:], AF.Exp, reads=['dl'], writes=['lg'], scale=-1.0)
        S.ts('dve', lg[:], lg[:], 1.0, None, ALU.add, reads=['lg'], writes=['lg'])
        S.act(lg[:], lg[:], AF.Ln, reads=['lg'], writes=['lg'])
        S.ts('dve', lg[:], lg[:], -1.0, None, ALU.mult, reads=['lg'], writes=['lg'])
        self.G128 = g = self.sb(st, "G128", [128, 8], F32)
        S.act(g[:], lg[:], AF.Exp, reads=['lg'], writes=['G128'], scale=128.0)
        la = self.sb(st, "la", [128, 256], F32)
        S.dma(la[:], self.dlam.partition_broadcast(128), writes=['la'])
        lv = la[:].rearrange("p (q two d) -> p q two d", q=2, two=2)
        pr = self.sb(st, "lapr", [128, 2, 64], F32)
        S.tt('dve', pr[:], lv[:, :, 0, :], lv[:, :, 1, :], ALU.mult, reads=['la'], writes=['lapr'])
        sm = self.sb(st, "lasm", [128, 4], F32)
        S.op('dve', lambda E: E.reduce_sum(sm[:, 0:2], pr[:], AX.X), reads=['lapr'], writes=['lasm'])
        S.act(sm[:, 2:4], sm[:, 0:2], AF.Exp, reads=['lasm'], writes=['lasm'])
        self.nlam = nl = self.sb(st, "nlam", [128, 1], F32)
        S.tt('dve', nl[:], sm[:, 3:4], sm[:, 2:3], ALU.subtract, reads=['lasm'], writes=['nlam'])
        S.ts('dve', nl[:], nl[:], -0.2, None, ALU.add, reads=['nlam'], writes=['nlam'])
        S.barrier()

    def l0_R(self):
        S, nc = self.S, self.nc
        self.drT = self.scr("drT", [8, 128, NQ], BF16)
        lg = self.lg
        with ExitStack() as p:
            Ds = self.sb(p, "Ds", [128, 4, 128], F32)
            dtmp = self.sb(p, "dtmp", [128, 2, 128], F32)
            for h in range(4):
                S.act(dtmp[:, 0, :], self.Af, AF.Exp, reads=['cf', 'lg'], writes=['dtmp0'], scale=lg[:, h:h + 1])
                S.act(dtmp[:, 1, :], self.Ab, AF.Exp, reads=['cf', 'lg'], writes=['dtmp1'], scale=lg[:, 4 + h:5 + h])
                S.tt('dve', dtmp[:, 0, :], dtmp[:, 0, :], self.Mf, ALU.mult, reads=['dtmp0'], writes=['dtmp0'])
                S.tt('dve', dtmp[:, 1, :], dtmp[:, 1, :], self.Mb, ALU.mult, reads=['dtmp1'], writes=['dtmp1'])
                S.tt('dve', Ds[:, h, :], dtmp[:, 0, :], dtmp[:, 1, :], ALU.add, reads=['dtmp0', 'dtmp1'], writes=['Ds'])
            rnw = self.sb(p, "rnw", [128, 1], F32)
            S.dma(rnw[:], self.rnw, writes=['rnw'])
            onesf = self.sb(p, "onesf", [128, 128], F32)
            S.memset('dve', onesf[:], 1.0, writes=['onesf'])
            Sb = self.sb(p, "Sb", [128, 2, NT_Q, 2, 128], BF16)
            Sm = Ring(nc, p, "Sm", 2, [128, 2, 128], F32)
            kdr = Ring(nc, p, "rkd", 3, [128, 256], BF16)
            vr = Ring(nc, p, "rvv", 3, [128, 512], BF16)
            pkv = Ring(nc, p, "pkv", 2, [128, 4, 128], F32, psum=True)
            fwd = list(range(NT_Q))
            bwd = [1, 0] + list(range(NT_ALL - 1, 1, -1))
            for dr, order, src in ((0, fwd, self.rkf), (1, bwd, self.rkb)):
                cur, ck = Sm.next()
                S.memset('dve', cur[:], 0.0, writes=[ck])
                for n in order:
                    if n < NT_Q:
                        S.copy('act', Sb[:, dr, n, :, :], cur[:], reads=[ck], writes=[('Sb', dr, n)])
                    if n == order[-1]:
                        break
                    kd, kk = kdr.next()
                    S.dma(kd[:], src[n * 128:(n + 1) * 128, :], writes=[kk])
                    v, vk = vr.next()
                    S.dma(v[:], self.rv0[n * 128:(n + 1) * 128, :], writes=[vk])
                    ps, pk = pkv.next()
                    for h in range(4):
                        c = h // 2
                        S.mm(ps[:, h, :], kd[:, c * 128:(c + 1) * 128], v[:, h * 128:(h + 1) * 128], True, True, reads=[kk, vk], writes=[pk])
                    nxt, nk = Sm.next()
                    for h in range(4):
                        c, hh = h // 2, h % 2
                        sl = slice(hh * 64, (hh + 1) * 64)
                        S.stt('dve', nxt[sl, c, :], cur[sl, c, :], self.G128[sl, dr * 4 + h:dr * 4 + h + 1], ps[sl, h, :], ALU.mult, ALU.add,
                              reads=[ck, pk, 'G128'], writes=[nk])
                    cur, ck = nxt, nk
            ktr = Ring(nc, p, "rkt", 2, [128, 2, 128], BF16)
            qr = Ring(nc, p, "rq3", 2, [128, 3, 2, 128], BF16)
            sgr = Ring(nc, p, "rsg", 2, [128, 4, 128], BF16)
            pTr = Ring(nc, p, "rpT", 3, [128, 128], BF16)
            pS = Ring(nc, p, "pS", 2, [128, 128], F32, psum=True)
            pO = Ring(nc, p, "pO", 2, [128, 4, 128], F32, psum=True)
            pN = Ring(nc, p, "pN", 1, [128, 512], F32, psum=True)
            osr = Ring(nc, p, "ros", 2, [128, 512], F32)
            sqr = Ring(nc, p, "rsq", 2, [128, 512], F32)
            rsr = Ring(nc, p, "rrs", 2, [128, 512], F32)
            rtr = Ring(nc, p, "rrt", 2, [128, 4, 128], BF16)
            for n in range(NT_Q):
                c0 = n * 128
                kt, ktk = ktr.next()
                S.dma(kt[:], self.rkT[:, :, c0:c0 + 128].rearrange("c p n -> p c n"), writes=[ktk])
                q3, qk = qr.next()
                for v_ in range(3):
                    S.dma(q3[:, v_, :, :], self.rqT[v_, :, :, c0:c0 + 128].rearrange("c p n -> p c n"), writes=[(qk, v_)])
                v, vk = vr.next()
                S.dma(v[:], self.rv0[c0:c0 + 128, :], writes=[vk])
                sg, sgk = sgr.next()
                S.dma(sg[:], self.sgT[:, :, c0:c0 + 128].rearrange("c p n -> p c n"), writes=[sgk])
                po, pok = pO.next()
                for h in range(4):
                    c, hh = h // 2, h % 2
                    sl = slice(hh * 64, (hh + 1) * 64)
                    ps, psk = pS.next()
                    S.mm(ps[:], kt[sl, c, :], q3[sl, 0, c, :], True, True, reads=[ktk, (qk, 0)], writes=[psk])
                    pT, pTk = pTr.next()
                    S.tt('dve', pT[:], ps[:], Ds[:, h, :], ALU.mult, reads=[psk, 'Ds'], writes=[pTk])
                    S.mm(po[:, h, :], v[:, h * 128:(h + 1) * 128], pT[:], True, False, reads=[vk, pTk], writes=[pok])
                    S.mm(po[:, h, :], Sb[sl, 0, n, c, :], q3[sl, 1, c, :], False, False, reads=[('Sb', 0, n), (qk, 1)], writes=[pok])
                    S.mm(po[:, h, :], Sb[sl, 1, n, c, :], q3[sl, 2, c, :], False, True, reads=[('Sb', 1, n), (qk, 2)], writes=[pok])
                osb, ok = osr.next()
                S.copy('act', osb[:], po[:].rearrange("p h n -> p (h n)"), reads=[pok], writes=[ok])
                sq, sqk = sqr.next()
                S.tt('pool', sq[:], osb[:], osb[:], ALU.mult, reads=[ok], writes=[sqk])
                pn, pnk = pN.next()
                S.mm(pn[:], onesf[:], sq[:], True, True, reads=[sqk, 'onesf'], writes=[pnk])
                rs, rk = rsr.next()
                S.ts('dve', rs[:], pn[:], 1.0 / 128, 1e-6, ALU.mult, ALU.add, reads=[pnk], writes=[rk])
                S.op('act', lambda E: E.sqrt(rs[:], rs[:]), reads=[rk], writes=[rk])
                S.op('dve', lambda E: E.reciprocal(rs[:], rs[:]), reads=[rk], writes=[rk])
                S.stt('dve', osb[:], osb[:], rnw[:, 0:1], rs[:], ALU.mult, ALU.mult, reads=[ok, rk, 'rnw'], writes=[ok])
                rt, rtk = rtr.next()
                S.tt('pool', rt[:].rearrange("p h n -> p (h n)"), osb[:], sg[:].rearrange("p h n -> p (h n)"), ALU.mult, reads=[ok, sgk], writes=[rtk])
                S.dma(self.drT[4:8, :, c0:c0 + 128].rearrange("c p n -> p c n"), rt[:], reads=[rtk], q='pool')
            S.barrier()

    def l0_D(self):
        S, nc = self.S, self.nc
        with ExitStack() as p:
            dn8 = self.sb(p, "dn8", [128, 1], F32)
            S.dma(dn8[:], self.dnw, writes=['dn8'])
            S.ts('dve', dn8[:], dn8[:], 0.8, None, ALU.mult, reads=['dn8'], writes=['dn8'])
            onesf = self.sb(p, "onesf2", [128, 128], F32)
            S.memset('dve', onesf[:], 1.0, writes=['onesf'])
            KTr = Ring(nc, p, "dKT", 2, [128, NE], BF16)
            Vr = Ring(nc, p, "dV", 2, [128, NT_ALL, 128], BF16)
            QTr = Ring(nc, p, "dQT", 2, [128, 512], BF16)
            pSr = Ring(nc, p, "dpS", 4, [128, 512], F32, psum=True)
            pO = [self.ps(p, f"dpO{c}", [128, 512], F32) for c in range(2)]
            pZ = [self.ps(p, f"dpZ{c}", [128, 512], F32) for c in range(2)]
            pTr = Ring(nc, p, "dpT", 5, [128, 512], BF16)
            zacc = [self.sb(p, f"dzacc{c}", [128, 512], F32) for c in range(2)]
            rzr = Ring(nc, p, "drz", 2, [128, 512], F32)
            tr_ = Ring(nc, p, "dtt", 3, [128, 512], F32)
            dor = Ring(nc, p, "ddo", 2, [128, 512], BF16)
            tiles = [(0, 256, [0, 1])] + [(256 + 512 * t, 512, list(range(NT_ALL))) for t in range(8)] + [(4352, 128, list(range(NT_ALL)))]
            QLIM = int(os.environ.get('QLIM', 99))
            tiles = tiles[:QLIM]
            for h in range(4):
                kt, ktk = KTr.next()
                S.dma(kt[:], self.kT0[h], writes=[ktk])
                v, vk = Vr.next()
                vsrc = self.v0[:, h * 128:(h + 1) * 128].rearrange("(c p) e -> p c e", p=128)
                for g in range(0, NT_ALL, 11):
                    S.dma(v[:, g:g + 11, :], vsrc[:, g:g + 11, :], writes=[(vk, g)])
                vkeys = [(vk, g) for g in range(0, NT_ALL, 11)]
                for (q0, N, chunks) in tiles:
                    qt, qk_ = QTr.next()
                    S.dma(qt[:, 0:N], self.qT0[h][:, q0:q0 + N], writes=[qk_])
                    last = len(chunks) - 1
                    units = [(idx, kc, c) for idx, kc in enumerate(chunks) for c in range(2)]
                    pend = []

                    def qk(u):
                        idx, kc, c = u
                        sl = slice(c * 64, (c + 1) * 64)
                        ps, psk = pSr.next()
                        S.mm(ps[:, 0:N], kt[sl, kc * 128:(kc + 1) * 128], qt[sl, 0:N], True, True, reads=[ktk, qk_], writes=[psk])
                        pT, pTk = pTr.next()
                        S.act(pT[:, 0:N], ps[:, 0:N], AF.Exp, reads=[psk], writes=[pTk], scale=0.125)
                        pend.append((u, pT, pTk))

                    def pv():
                        (idx, kc, c), pT, pTk = pend.pop(0)
                        S.mm(pO[c][:, 0:N], v[:, kc, :], pT[:, 0:N], idx == 0, idx == last, reads=[pTk] + vkeys, writes=[('dO', c)])
                        if idx == 0:
                            S.copy('dve', zacc[c][:, 0:N], pT[:, 0:N], reads=[pTk], writes=[('zacc', c)])
                        else:
                            S.tt('dve', zacc[c][:, 0:N], zacc[c][:, 0:N], pT[:, 0:N], ALU.add, reads=[pTk, ('zacc', c)], writes=[('zacc', c)])
                        if idx == last:
                            S.mm(pZ[c][:, 0:N], onesf[:], zacc[c][:, 0:N], True, True, reads=[('zacc', c), 'onesf'], writes=[('dZ', c)])

                    LOOK = 3
                    for ui, u in enumerate(units):
                        qk(u)
                        if ui >= LOOK:
                            pv()
                    while pend:
                        pv()
                    tt = []
                    for c in range(2):
                        rz, rzk = rzr.next()
                        S.op('dve', lambda E: E.reciprocal(rz[:, 0:N], pZ[c][:, 0:N]), reads=[('dZ', c)], writes=[rzk, ('dZ', c)])
                        t_, tk = tr_.next()
                        S.tt('dve', t_[:, 0:N], pO[c][:, 0:N], rz[:, 0:N], ALU.mult, reads=[('dO', c), rzk], writes=[tk, ('dO', c)])
                        tt.append((t_, tk))
                    o, ok = tr_.next()
                    S.stt('dve', o[:, 0:N], tt[1][0][:, 0:N], self.nlam[:, 0:1], tt[0][0][:, 0:N], ALU.mult, ALU.add,
                          reads=[tt[0][1], tt[1][1], 'nlam'], writes=[ok])
                    sq, sqk = rzr.next()
                    S.tt('pool', sq[:, 0:N], o[:, 0:N], o[:, 0:N], ALU.mult, reads=[ok], writes=[sqk])
                    pN, pNk = pSr.next()
                    S.mm(pN[:, 0:N], onesf[:], sq[:, 0:N], True, True, reads=[sqk, 'onesf'], writes=[pNk])
                    rs, rk = rzr.next()
                    S.ts('dve', rs[:, 0:N], pN[:, 0:N], 1.0 / 128, 1e-6, ALU.mult, ALU.add, reads=[pNk], writes=[rk, pNk])
                    S.op('act', lambda E: E.sqrt(rs[:, 0:N], rs[:, 0:N]), reads=[rk], writes=[rk])
                    S.op('dve', lambda E: E.reciprocal(rs[:, 0:N], rs[:, 0:N]), reads=[rk], writes=[rk])
                    do, dk = dor.next()
                    S.stt('dve', do[:, 0:N], o[:, 0:N], dn8[:, 0:1], rs[:, 0:N], ALU.mult, ALU.mult, reads=[ok, rk, 'dn8'], writes=[dk])
                    S.dma(self.drT[h][:, q0:q0 + N], do[:, 0:N], reads=[dk], q='pool')
            S.barrier()

    def post_init(self, p, l, ntok):
        S, nc = self.S, self.nc
        R = {}
        R['rwb'] = rwb = self.sb(p, "rwb", [128, 8, 32], BF16)
        rwf = self.sb(p, "rwf", [128, 8, 32], F32)
        S.dma(rwf[:], self.rw[l].rearrange("(k p) e -> p k e", p=128), writes=['rwf'])
        S.copy('dve', rwb[:], rwf[:], reads=['rwf'], writes=['rwb'])
        R['rbt'] = rbt = self.sb(p, "rbt", [128, 32], F32)
        S.dma(rbt[:], self.rb[l:l + 1, :].partition_broadcast(128), writes=['rbt'])
        R['xr'] = Ring(nc, p, "pxt", 2, [128, 1024], F32)
        R['tm'] = Ring(nc, p, "ptm", 2, [128, 1024], F32)
        R['h1'] = Ring(nc, p, "ph1", 2, [128, 1024], F32)
        R['rs'] = Ring(nc, p, "prs", 3, [128, 4], F32)
        R['tmp'] = Ring(nc, p, "ptmp", 2, [128, 1024], F32)
        R['ab'] = Ring(nc, p, "pab", 2, [128, 1024], BF16)
        R['aT'] = Ring(nc, p, "paT", 3, [128, 8, 128], BF16)
        R['pT'] = Ring(nc, p, "ppT", 1, [128, 8, 128], BF16, psum=True)
        R['pL'] = Ring(nc, p, "ppL", 1, [128, 512], F32, psum=True)
        R['lg'] = Ring(nc, p, "plg", 2, [128, 32], F32)
        R['sm'] = Ring(nc, p, "psm", 2, [128, 16], F32)
        R['eg'] = Ring(nc, p, "peg", 2, [128, 3, 32], F32)
        R['gt'] = Ring(nc, p, "pgt", 2, [32, 128], F32)
        return R

    def post_mixer(self, R, i, r0, t, MT, mps, hsrc, h1d, fTd, gTd, fTokd=None, gTokd=None):
        S = self.S
        xt, xk = R['xr'].next()
        S.dma(xt[:], hsrc, writes=[xk])
        tm, tmk = R['tm'].next()
        for n in range(2):
            S.tt('dve', tm[:, n * 512:(n + 1) * 512], mps[n][0][:], MT[t][:, 2048 + n * 512:2048 + (n + 1) * 512], ALU.mult,
                 reads=[mps[n][1]], writes=[(tmk, n), mps[n][1]])
        h1, h1k = R['h1'].next()
        S.tt('pool', h1[:], tm[:], xt[:], ALU.add, reads=[(tmk, 0), (tmk, 1), xk], writes=[h1k])
        S.dma(h1d[r0:r0 + 128, :], h1[:], reads=[h1k], q='pool')
        aT, aTk = self.front(h1[:], h1k, MT[t][:, 4096:5120], MT[t][:, 3072:4096], R['rs'], R['tmp'], R['ab'], R['pT'], R['aT'])
        S.dma(fTd[:, :, r0:r0 + 128].rearrange("k p n -> p k n"), aT[:], reads=[aTk], q='pool')
        if fTokd is not None:
            ab, abk = self.last_ab
            S.dma(fTokd[r0:r0 + 128, :], ab[:], reads=[abk], q='pool')
        pl, plk = R['pL'].next()
        for k in range(8):
            S.mm(pl[:, 0:32], aT[:, k, :], R['rwb'][:, k, :], k == 0, k == 7, reads=[aTk, 'rwb'], writes=[plk])
        lg, lgk = R['lg'].next()
        S.tt('dve', lg[:], pl[:, 0:32], R['rbt'][:], ALU.add, reads=[plk, 'rbt'], writes=[lgk, plk])
        sm, smk = R['sm'].next()
        S.op('dve', lambda E: E.max(sm[:, 0:8], lg[:]), reads=[lgk], writes=[smk])
        eg, egk = R['eg'].next()
        S.ts('dve', eg[:, 0, :], lg[:], sm[:, 3:4], None, ALU.is_ge, reads=[lgk, smk], writes=[(egk, 0)])
        S.ts('dve', sm[:, 8:9], sm[:, 0:1], -1.0, None, ALU.mult, reads=[smk], writes=[smk])
        S.act(eg[:, 1, :], lg[:], AF.Exp, reads=[lgk, smk], writes=[(egk, 1)], bias=sm[:, 8:9])
        S.tt('dve', eg[:, 1, :], eg[:, 1, :], eg[:, 0, :], ALU.mult, reads=[(egk, 0), (egk, 1)], writes=[(egk, 1)])
        S.op('dve', lambda E: E.reduce_sum(sm[:, 9:10], eg[:, 1, :], AX.X), reads=[(egk, 1), smk], writes=[smk])
        S.op('dve', lambda E: E.reciprocal(sm[:, 10:11], sm[:, 9:10]), reads=[smk], writes=[smk])
        S.ts('dve', eg[:, 2, :], eg[:, 1, :], sm[:, 10:11], None, ALU.mult, reads=[(egk, 1), smk], writes=[(egk, 2)])
        if gTokd is not None:
            S.dma(gTokd[r0:r0 + 128, :], eg[:, 2, :], reads=[(egk, 2)], q='pool')
        pg, pgk = R['pL'].next()
        S.tr(pg[0:32, 0:128], eg[:, 2, :], self.idf, reads=[(egk, 2), 'cf'], writes=[pgk])
        gt, gtk = R['gt'].next()
        S.copy('act', gt[:], pg[0:32, 0:128], reads=[pgk], writes=[gtk, pgk])
        S.dma(gTd[:, r0:r0 + 128], gt[:], reads=[gtk], q='pool')

    def l0_F(self, MT):
        S, nc = self.S, self.nc
        self.h1 = self.scr("h1", [NQ, 1024], F32)
        self.fT0 = self.scr("fT0", [8, 128, NQ], BF16)
        self.gT0 = self.scr("gT0", [32, NQ], F32)
        self.fTok0 = self.scr("fTok0", [NQ, 1024], BF16)
        self.gTok0 = self.scr("gTok0", [NQ, 32], F32)
        with ExitStack() as p:
            stg = Ring(nc, p, "stgF", 2, [128, 1024], F32)
            wo = self.load_w_bf16(p, "wo0", self.w_out0, 1024, stg)
            R = self.post_init(p, 0, NQ)
            drr = Ring(nc, p, "fdr", 3, [128, 8, 128], BF16)
            pM = Ring(nc, p, "fpM", 4, [128, 512], F32, psum=True)
            for i in range(NT_Q):
                r0 = i * 128
                t = 1 if i < 2 else 0
                dr, drk = drr.next()
                S.dma(dr[:], self.drT[:, :, r0:r0 + 128].rearrange("k p n -> p k n"), writes=[drk])
                mps = []
                for n in range(2):
                    ps, pk = pM.next()
                    for k in range(8):
                        S.mm(ps[:], dr[:, k, :], wo[:, k, n * 512:(n + 1) * 512], k == 0, k == 7, reads=[drk], writes=[pk])
                    mps.append((ps, pk))
                self.post_mixer(R, i, r0, t, MT, mps, self.xl[r0:r0 + 128, :], self.h1, self.fT0, self.gT0, self.fTok0, self.gTok0)
            S.barrier()

    def rope_tm(self, src, srck, H, rp, rpk, r1, dst, dstk):
        S = self.S
        xv = src.rearrange("p (h r a f) -> p h r a f", h=H, r=2, a=2, f=16)
        a, b = xv[:, :, :, 0, :], xv[:, :, :, 1, :]
        cos = rp[:, 0:32].rearrange("p (r f) -> p r f", r=2).unsqueeze(1).to_broadcast([128, H, 2, 16])
        sin = rp[:, 32:64].rearrange("p (r f) -> p r f", r=2).unsqueeze(1).to_broadcast([128, H, 2, 16])
        t, tk = r1.next()
        tv = [t[:, i, 0:H * 32].rearrange("p (h r f) -> p h r f", h=H, r=2) for i in range(4)]
        S.tt('dve', tv[0], a, cos, ALU.mult, reads=[srck, rpk], writes=[(tk, 0)])
        S.tt('dve', tv[1], b, sin, ALU.mult, reads=[srck, rpk], writes=[(tk, 1)])
        S.tt('dve', tv[2], a, sin, ALU.mult, reads=[srck, rpk], writes=[(tk, 2)])
        S.tt('dve', tv[3], b, cos, ALU.mult, reads=[srck, rpk], writes=[(tk, 3)])
        qv = dst.rearrange("p (h r a f) -> p h r a f", h=H, r=2, a=2, f=16)
        S.tt('pool', qv[:, :, :, 0, :], tv[0], tv[1], ALU.subtract, reads=[(tk, 0), (tk, 1)], writes=[(dstk, 0)])
        S.tt('pool', qv[:, :, :, 1, :], tv[2], tv[3], ALU.add, reads=[(tk, 2), (tk, 3)], writes=[(dstk, 1)])

    def l1_A(self, MT):
        S, nc = self.S, self.nc
        self.qT1 = self.scr("qT1", [8, 128, 4096], BF16)
        self.kTd1 = self.scr("kTd1", [2, 128, NQ], BF16)
        self.v1 = self.scr("v1", [NQ, 128], BF16)
        with ExitStack() as p:
            stg = Ring(nc, p, "stg1", 2, [128, 1024], F32)
            w = self.load_w_bf16(p, "wqkv", self.w_qkv, 1280, stg)
            xr = Ring(nc, p, "axt", 3, [128, 1024], F32)
            rpr = Ring(nc, p, "arp", 3, [128, 64], F32)
            rsr = Ring(nc, p, "ars", 3, [128, 4], F32)
            tmr = Ring(nc, p, "atmp", 2, [128, 1024], F32)
            abr = Ring(nc, p, "aab", 2, [128, 1024], BF16)
            aTr = Ring(nc, p, "aaT", 2, [128, 8, 128], BF16)
            pTr = Ring(nc, p, "apT", 2, [128, 8, 128], BF16, psum=True)
            pPr = Ring(nc, p, "apP", 3, [128, 512], F32, psum=True)
            pQr = Ring(nc, p, "apQ", 2, [128, 8, 128], BF16, psum=True)
            r1 = Ring(nc, p, "ar1", 2, [128, 4, 256], F32)
            qrr = Ring(nc, p, "aqr", 2, [128, 1024], BF16)
            krr = Ring(nc, p, "akr", 2, [128, 128], BF16)
            kdr = Ring(nc, p, "akd", 2, [128, 2, 128], BF16)
            qTs = Ring(nc, p, "aqT", 2, [128, 8, 128], BF16)
            kTs = Ring(nc, p, "akT", 2, [128, 2, 128], BF16)
            vbr = Ring(nc, p, "avb", 2, [128, 128], BF16)
            for i in range(NT_Q):
                t = 1 if i < 2 else 0
                r0 = i * 128
                xt, xk = xr.next()
                S.dma(xt[:], self.h2[r0:r0 + 128, :], writes=[xk])
                rp, rpk = rpr.next()
                S.dma(rp[:], self.rope[r0:r0 + 128, :], writes=[rpk])
                aT, aTk = self.front(xt[:], xk, MT[t][:, 1024:2048], MT[t][:, 0:1024], rsr, tmr, abr, pTr, aTr)
                ps, pk = pPr.next()
                for k in range(8):
                    S.mm(ps[:, 0:256], aT[:, k, :], w[:, k, 1024:1280], k == 0, k == 7, reads=[aTk], writes=[pk])
                kr, krk = krr.next()
                self.rope_tm(ps[:, 0:128], pk, 2, rp, rpk, r1, kr[:], krk)
                vb, vk = vbr.next()
                S.copy('act', vb[:], ps[:, 128:256], reads=[pk], writes=[vk, pk])
                S.dma(self.v1[r0:r0 + 128, :], vb[:], reads=[vk], q='pool')
                kd, kdk = kdr.next()
                for kvh in range(2):
                    S.copy('pool', kd[:, kvh, :].rearrange("p (two d) -> p two d", two=2),
                           kr[:, kvh * 64:(kvh + 1) * 64].unsqueeze(1).to_broadcast([128, 2, 64]), reads=[(krk, 0), (krk, 1)], writes=[(kdk, kvh)])
                pq, pqk = pQr.next()
                for kvh in range(2):
                    S.tr(pq[:, kvh, :], kd[:, kvh, :], self.idb[:], reads=[(kdk, kvh)], writes=[pqk])
                kT, kTk = kTs.next()
                S.copy('act', kT[:], pq[:, 0:2, :], reads=[pqk], writes=[kTk, pqk])
                S.dma(self.kTd1[:, :, r0:r0 + 128].rearrange("c p n -> p c n"), kT[:], reads=[kTk], q='pool')
                if 2 <= i < 34:
                    qr, qrk = qrr.next()
                    for g in range(2):
                        ps, pk = pPr.next()
                        for k in range(8):
                            S.mm(ps[:], aT[:, k, :], w[:, k, g * 512:(g + 1) * 512], k == 0, k == 7, reads=[aTk], writes=[pk])
                        self.rope_tm(ps[:], pk, 8, rp, rpk, r1, qr[:, g * 512:(g + 1) * 512], (qrk, g))
                    pq, pqk = pQr.next()
                    for c in range(8):
                        S.tr(pq[:, c, :], qr[:, c * 128:(c + 1) * 128], self.idb[:], reads=[((qrk, c // 4), 0), ((qrk, c // 4), 1)], writes=[pqk])
                    qT, qTk = qTs.next()
                    S.copy('act', qT[:], pq[:], reads=[pqk], writes=[qTk, pqk])
                    q0 = (i - 2) * 128
                    S.dma(self.qT1[:, :, q0:q0 + 128].rearrange("c p n -> p c n"), qT[:], reads=[qTk], q='pool')
            S.barrier()

    def l1_W(self, MT):
        S, nc = self.S, self.nc
        self.h3 = self.scr("h3", [4096, 1024], F32)
        self.fT1 = self.scr("fT1", [8, 128, 4096], BF16)
        self.gT1 = self.scr("gT1", [32, 4096], F32)
        self.fTok1 = self.scr("fTok1", [4096, 1024], BF16)
        self.gTok1 = self.scr("gTok1", [4096, 32], F32)
        with ExitStack() as p:
            wo = self.sb(p, "wo1", [64, 16, 1024], BF16)
            stg = Ring(nc, p, "stgW", 2, [64, 1024], F32)
            wv = self.w_out1.rearrange("(h e) n -> e h n", e=64)
            for g in range(16):
                st_, sk = stg.next()
                S.dma(st_[:], wv[:, g, :], writes=[sk])
                S.copy('pool', wo[:, g, :], st_[:], reads=[sk], writes=[('wo1x', g)])
            S.barrier()
            KT = self.sb(p, "wKT", [128, 2, NQ], BF16)
            S.dma(KT[:], self.kTd1.rearrange("c p n -> p c n"), writes=['wKT'])
            V = self.sb(p, "wV", [128, NT_Q, 128], BF16)
            S.dma(V[:], self.v1.rearrange("(c p) e -> p c e", p=128), writes=['wV'])
            ES = self.sb(p, "wES", [128, 16], F32)
            S.dma(ES[:], self.sinks.partition_broadcast(128), writes=['wES'])
            S.act(ES[:], ES[:], AF.Exp, reads=['wES'], writes=['wES'])
            S.barrier()
            R = self.post_init(p, 1, 4096)
            QTr = Ring(nc, p, "wQT", 2, [128, 8, 128], BF16)
            pS = Ring(nc, p, "wpS", 2, [128, 512], F32, psum=True)
            pO = self.ps(p, "wpO", [128, 512], F32)
            pZ = self.ps(p, "wpZ", [128, 512], F32)
            pM = Ring(nc, p, "wpM", 2, [128, 512], F32, psum=True)
            pTr = Ring(nc, p, "wpT", 3, [128, 4, 128], BF16)
            zr = Ring(nc, p, "wz", 2, [64, 4, 128], F32)
            oTr = Ring(nc, p, "woT", 2, [64, 16, 128], BF16)
            for n in range(32):
                iq = n + 2
                qt, qk = QTr.next()
                S.dma(qt[:], self.qT1[:, :, n * 128:(n + 1) * 128].rearrange("c p n -> p c n"), writes=[qk])
                oT, oTk = oTr.next()
                chunks = [(0, None), (1, None)]
                if iq - 1 >= 2:
                    chunks.append((iq - 1, self.Mbb))
                chunks.append((iq, None))
                if iq + 1 <= 34:
                    chunks.append((iq + 1, self.Mfb))
                last = len(chunks) - 1
                for kvh in range(2):
                    for hh in range(2):
                        sl = slice(hh * 64, (hh + 1) * 64)
                        slot0 = (kvh * 2 + hh) * 4
                        for idx, (kt, mask) in enumerate(chunks):
                            ps, psk = pS.next()
                            S.mm(ps[:], KT[sl, kvh, kt * 128:(kt + 1) * 128], qt[sl, kvh * 4:(kvh + 1) * 4, :], True, True, reads=['wKT', qk], writes=[psk])
                            pT, pTk = pTr.next()
                            S.act(pT[:].rearrange("p h n -> p (h n)"), ps[:], AF.Exp, reads=[psk], writes=[pTk], scale=0.125)
                            if mask is not None:
                                S.tt('pool', pT[:], pT[:], mask[:].unsqueeze(1).to_broadcast([128, 4, 128]), ALU.mult, reads=[pTk], writes=[pTk])
                            pTf = pT[:].rearrange("p h n -> p (h n)")
                            S.mm(pO[0:64, :], V[:, kt, kvh * 64:(kvh + 1) * 64], pTf, idx == 0, idx == last, reads=[pTk, 'wV'], writes=['wpO'])
                            S.mm(pZ[0:64, :], self.onesb[:, 0:64], pTf, idx == 0, idx == last, reads=[pTk, 'onesb'], writes=['wpZ'])
                        z, zk = zr.next()
                        es = ES[0:64, kvh * 8 + hh:kvh * 8 + 8:2].unsqueeze(2).to_broadcast([64, 4, 128])
                        S.tt('dve', z[:], pZ[0:64, :].rearrange("p (h n) -> p h n", h=4), es, ALU.add, reads=['wpZ', 'wES'], writes=[zk, 'wpZ'])
                        S.op('dve', lambda E: E.reciprocal(z[:], z[:]), reads=[zk], writes=[zk])
                        S.tt('dve', oT[:, slot0:slot0 + 4, :], pO[0:64, :].rearrange("p (h n) -> p h n", h=4), z[:], ALU.mult,
                             reads=['wpO', zk], writes=[(oTk, slot0), 'wpO'])
                mps = []
                for nn in range(2):
                    pm, pmk = pM.next()
                    for slot in range(16):
                        kvh, hh, c = slot // 8, (slot // 4) % 2, slot % 4
                        qh = kvh * 8 + 2 * c + hh
                        S.mm(pm[:], oT[:, slot, :], wo[:, qh, nn * 512:(nn + 1) * 512], slot == 0, slot == 15,
                             reads=[(oTk, (slot // 4) * 4)], writes=[pmk])
                    mps.append((pm, pmk))
                self.post_mixer(R, n, n * 128, 0, MT, mps, self.h2[iq * 128:(iq + 1) * 128, :], self.h3, self.fT1, self.gT1, self.fTok1, self.gTok1)
            S.barrier()

    def moe2(self, l, G2, fTd, gTd, fTokd, gTokd, h1d, ntiles, per_pass, ctx_tiles, sink):
        S, nc = self.S, self.nc
        NEX = int(os.environ.get('NEX', 32))
        FORCE = os.environ.get('MOE_FORCE')
        I32 = mybir.dt.int32
        with ExitStack() as p:
            TP = per_pass * 128
            NSUB = (per_pass + 3) // 4
            fT = self.sb(p, "mfT", [128, 8, TP], BF16)
            fTok = self.sb(p, "mfTok", [128, per_pass, 1024], BF16)
            gT = self.sb(p, "mgT", [32, TP], F32)
            gtok = self.sb(p, "mgtok", [128, per_pass, 32], F32)
            mask = self.sb(p, "mmask", [128, per_pass, 32], F32)
            pos1 = self.sb(p, "mpos1", [128, per_pass, 32], F32)
            flagf = self.sb(p, "mflagf", [128, 32], F32)
            flagi = self.sb(p, "mflagi", [128, 32], I32)
            tris = self.sb(p, "mtris", [128, 128], F32)
            onesf = self.sb(p, "monesf", [128, 128], F32)
            S.tt('dve', tris[:], self.Mf, self.idf, ALU.subtract, reads=['cf'], writes=['mtris'])
            S.memset('dve', onesf[:], 1.0, writes=['monesf'])
            yacc = self.sb(p, "myacc", [128, per_pass, 1024], F32)
            w1r = Ring(nc, p, "mw1", 2, [128, 8, 2048], BF16)
            w2r = Ring(nc, p, "mw2", 2, [128, 8, 1024], BF16)
            b1r = Ring(nc, p, "mb1", 2, [128, 24], F32)
            selr = Ring(nc, p, "msel", 2, [32, 128], F32)
            gB = self.sb(p, "mgB", [128, 256], F32)
            gcr = Ring(nc, p, "mgc", 2, [128, 256], F32)
            sgr = Ring(nc, p, "msg", 2, [128, 256], F32)
            l1r = Ring(nc, p, "ml1", 2, [128, 256], F32)
            ac = self.sb(p, "mac", [128, 8, 512], BF16)
            hxv = ac[:].rearrange("p k n -> p (k n)").bitcast(F32)[:, 0:1024]
            b2s = ac[0:32].rearrange("p k n -> p (k n)").bitcast(F32)[:, 1024:2048]
            CAP = 256
            Dt = self.sb(p, "mD", [128, per_pass, CAP], BF16)
            Dg = self.sb(p, "mDg", [128, 2, CAP], BF16)
            DgT = self.sb(p, "mDgT", [128, 2, per_pass, 128], BF16)
            XcT = self.sb(p, "mXcT", [128, 8, CAP], BF16)
            Ycb = self.sb(p, "mYcb", [128, 2, 1024], BF16)
            ioc = self.sb(p, "mioc", [128, CAP], F32)
            S.copy('dve', ioc[:, 0:128], self.IO1, reads=['cf'], writes=['mioc'])
            S.ts('dve', ioc[:, 128:256], self.IO1, 128.0, None, ALU.add, reads=['cf'], writes=['mioc'])
            pH = [self.ps(p, f"mpH{i}", [128, 512], F32) for i in range(4)]
            pY = [self.ps(p, f"mpY{i}", [128, 512], F32) for i in range(2)]
            pG = self.ps(p, "mpG", [128, 512], F32)
            pT8 = self.ps(p, "mpT8", [128, 4, 128], BF16)
            w1v = lambda e: self.w1[l, e].rearrange("(k p) n -> p k n", p=128)
            w2v = lambda e: self.w2[l, e].rearrange("(k p) n -> p k n", p=128)

            def load_expert(e):
                w1b, w1k = w1r.next()
                w2b, w2k = w2r.next()
                for k in range(8):
                    S.dma(w1b[:, k, :], w1v(e)[:, k, :], writes=[(w1k, k)], q='pool')
                for k in range(0, 8, 2):
                    S.dma(w2b[:, k:k + 2, :], w2v(e)[:, k:k + 2, :], writes=[(w2k, k)], q='pool')
                b1, b1k = b1r.next()
                S.dma(b1[:, 0:16], self.b1[l, e], writes=[b1k])
                S.ts('dve', b1[:, 16:24], b1[:, 8:16], 1.0, None, ALU.add, reads=[b1k], writes=[b1k])
                sl, slk = selr.next()
                S.dma(sl[:], self.sel[:, e * 128:(e + 1) * 128], writes=[slk])
                keys = [(w1k, k) for k in range(8)] + [(w2k, k) for k in range(0, 8, 2)] + [b1k, slk]
                return (w1b, w2b, b1, sl, keys)

            class Rot:
                def __init__(self, items):
                    self.items, self.i = items, 0

                def next(self):
                    j = self.i % len(self.items)
                    self.i += 1
                    return self.items[j], ('rot', id(self.items), j)

            def chain_head(Q, hg, hgk, hl, hlk, b1, j, N):
                gc, gck = gcr.next()
                Q.ts('dve', gc[:, 0:N], hg[:, 0:N], b1[:, j:j + 1], 7.0, ALU.add, ALU.min, reads=[hgk], writes=[gck, hgk])
                sg, sgk = sgr.next()
                Q.act(sg[:, 0:N], gc[:, 0:N], AF.Sigmoid, reads=[gck], writes=[sgk], scale=1.702)
                l1, l1k = l1r.next()
                Q.ts('dve', l1[:, 0:N], hl[:, 0:N], b1[:, 16 + j:17 + j], 8.0, ALU.add, ALU.min, reads=[hlk], writes=[l1k, hlk])
                Q.tt('pool', sg[:, 0:N], gc[:, 0:N], sg[:, 0:N], ALU.mult, reads=[gck, sgk], writes=[sgk])
                return (sg, sgk, l1, l1k)

            def chain_tail(Q, st, N, out_ap, outk, gBap):
                sg, sgk, l1, l1k = st
                if gBap is None:
                    Q.stt('dve', out_ap, l1[:, 0:N], -6.0, sg[:, 0:N], ALU.max, ALU.mult, reads=[l1k, sgk], writes=[outk])
                else:
                    Q.stt('dve', l1[:, 0:N], l1[:, 0:N], -6.0, sg[:, 0:N], ALU.max, ALU.mult, reads=[l1k, sgk], writes=[l1k])
                    Q.tt('pool', out_ap, l1[:, 0:N], gBap, ALU.mult, reads=[l1k, 'gB'], writes=[outk])

            def dense_unit(Q, e, W, c0, N):
                w1b, w2b, b1, sl, _ = W
                pHr = Rot(pH)
                pYr = Rot(pY)
                Q.mm(pG[:, 0:N], sl[0:32, :], gT[0:32, c0:c0 + N], True, True, writes=['pG'])
                Q.copy('act', gB[:, 0:N], pG[:, 0:N], reads=['pG'], writes=['gB', 'pG'])
                prev = None
                for j in range(8):
                    hg, hgk = pHr.next()
                    for k in range(8):
                        Q.mm(hg[:, 0:N], w1b[:, k, j * 128:(j + 1) * 128], fT[:, k, c0:c0 + N], k == 0, k == 7, writes=[hgk])
                    hl, hlk = pHr.next()
                    for k in range(8):
                        Q.mm(hl[:, 0:N], w1b[:, k, 1024 + j * 128:1024 + (j + 1) * 128], fT[:, k, c0:c0 + N], k == 0, k == 7, writes=[hlk])
                    st = chain_head(Q, hg, hgk, hl, hlk, b1, j, N)
                    if prev is not None:
                        chain_tail(Q, prev[0], N, ac[:, prev[1], 0:N], ('ac', prev[1]), gB[:, 0:N])
                    prev = (st, j)
                chain_tail(Q, prev[0], N, ac[:, prev[1], 0:N], ('ac', prev[1]), gB[:, 0:N])
                for s_ in range(N // 128):
                    ti = (c0 // 128) + s_
                    for n in range(2):
                        py, pyk = pYr.next()
                        for j in range(8):
                            Q.mm(py[:], ac[:, j, s_ * 128:(s_ + 1) * 128], w2b[:, j, n * 512:(n + 1) * 512], j == 0, j == 7,
                                 reads=[('ac', j)], writes=[pyk])
                        ysl = yacc[:, ti, n * 512:(n + 1) * 512]
                        Q.tt('dve', ysl, py[:], ysl, ALU.add, reads=[pyk], writes=[('y', ti, n), pyk])

            def sparse_unit(Q, e, W, nt):
                w1b, w2b, b1, sl, _ = W
                pHr = Rot(pH)
                pYr = Rot(pY)
                for t in range(nt):
                    Q.ts('dve', Dt[:, t, :], ioc[:], pos1[:, t, e:e + 1], mask[:, t, e:e + 1], ALU.is_equal, ALU.mult, writes=[('D', t)])
                    dgs = t % 2
                    Q.ts('dve', Dg[:, dgs, :], ioc[:], pos1[:, t, e:e + 1], gtok[:, t, e:e + 1], ALU.is_equal, ALU.mult, writes=[('Dg', dgs)])
                    for sc in range(2):
                        Q.tr(pT8[:, dgs * 2 + sc, :], Dg[:, dgs, sc * 128:(sc + 1) * 128], self.idb[:], reads=[('Dg', dgs)], writes=[('pT8', dgs)])
                    Q.copy('dve', DgT[:, :, t, :], pT8[:, dgs * 2:dgs * 2 + 2, :], reads=[('pT8', dgs)], writes=[('DgT', t), ('pT8', dgs)])
                for kp in range(4):
                    ps, psk = pHr.next()
                    for kk in range(2):
                        k = kp * 2 + kk
                        for t in range(nt):
                            Q.mm(ps[:, kk * CAP:(kk + 1) * CAP], fTok[:, t, k * 128:(k + 1) * 128], Dt[:, t, :], t == 0, t == nt - 1,
                                 reads=[('D', t)], writes=[psk])
                    Q.copy('dve', XcT[:, kp * 2:kp * 2 + 2, :].rearrange("p k n -> p (k n)"), ps[:], reads=[psk], writes=[('Xc', kp), psk])
                prev = None
                for j in range(8):
                    hg, hgk = pHr.next()
                    for k in range(8):
                        Q.mm(hg[:, 0:CAP], w1b[:, k, j * 128:(j + 1) * 128], XcT[:, k, :], k == 0, k == 7, reads=[('Xc', k // 2)], writes=[hgk])
                    hl, hlk = pHr.next()
                    for k in range(8):
                        Q.mm(hl[:, 0:CAP], w1b[:, k, 1024 + j * 128:1024 + (j + 1) * 128], XcT[:, k, :], k == 0, k == 7, reads=[('Xc', k // 2)], writes=[hlk])
                    st = chain_head(Q, hg, hgk, hl, hlk, b1, j, CAP)
                    if prev is not None:
                        chain_tail(Q, prev[0], CAP, ac[:, prev[1], 0:CAP], ('ac', prev[1]), None)
                    prev = (st, j)
                chain_tail(Q, prev[0], CAP, ac[:, prev[1], 0:CAP], ('ac', prev[1]), None)
                for sc in range(2):
                    for n in range(2):
                        py, pyk = pYr.next()
                        for j in range(8):
                            Q.mm(py[:], ac[:, j, sc * 128:(sc + 1) * 128], w2b[:, j, n * 512:(n + 1) * 512], j == 0, j == 7, reads=[('ac', j)], writes=[pyk])
                        Q.copy('dve', Ycb[:, sc, n * 512:(n + 1) * 512], py[:], reads=[pyk], writes=[('Yc', sc, n), pyk])
                for n in range(2):
                    for t in range(nt):
                        ps, psk = pHr.next()
                        for sc in range(2):
                            Q.mm(ps[:], DgT[:, sc, t, :], Ycb[:, sc, n * 512:(n + 1) * 512], sc == 0, sc == 1,
                                 reads=[('DgT', t), ('Yc', sc, n)], writes=[psk])
                        ysl = yacc[:, t, n * 512:(n + 1) * 512]
                        Q.tt('dve', ysl, ps[:], ysl, ALU.add, reads=[psk], writes=[('y', t, n), psk])

            t0 = 0
            while t0 < ntiles:
                nt = min(per_pass, ntiles - t0)
                T = nt * 128
                c_base = t0 * 128
                S.dma(fT[:, :, 0:T], fTd[:, :, c_base:c_base + T].rearrange("k p n -> p k n"), writes=['mfT'])
                S.dma(gT[:, 0:T], gTd[:, c_base:c_base + T], writes=['mgT'])
                S.dma(b2s, self.b2[l], writes=['mb2'])
                S.dma(fTok[:, 0:nt, :], fTokd[c_base:c_base + T, :].rearrange("(t p) d -> p t d", p=128), writes=['mfTok'])
                S.dma(gtok[:, 0:nt, :], gTokd[c_base:c_base + T, :].rearrange("(t p) e -> p t e", p=128), writes=['mgtok'])
                S.ts('dve', mask[:, 0:nt, :], gtok[:, 0:nt, :], 0.0, None, ALU.is_gt, reads=['mgtok'], writes=['mmask'])
                subs = [(c, min(256, T - c)) for c in range(0, T, 256)]
                for t in range(nt):
                    S.mm(pG[:, 0:32], tris[:], mask[:, t, :], True, t == 0, reads=['mmask', 'mtris'], writes=['pG'])
                    for t2 in range(t):
                        S.mm(pG[:, 0:32], onesf[:], mask[:, t2, :], False, t2 == t - 1, reads=['mmask', 'monesf'], writes=['pG'])
                    S.ts('dve', pos1[:, t, :], pG[:, 0:32], 1.0, None, ALU.add, reads=['pG'], writes=[('pos1', t), 'pG'])
                for t in range(nt):
                    S.mm(pG[:, 0:32], onesf[:], mask[:, t, :], t == 0, t == nt - 1, reads=['mmask', 'monesf'], writes=['pG'])
                S.ts('dve', flagf[:], pG[:, 0:32], float(CAP), None, ALU.is_gt, reads=['pG'], writes=['mflagf', 'pG'])
                S.copy('dve', flagi[:], flagf[:], reads=['mflagf'], writes=['mflagi'])
                for s_ in range(nt):
                    for n in range(2):
                        ps = pY[n]
                        S.mm(ps[:], gT[0:32, s_ * 128:(s_ + 1) * 128], b2s[:, n * 512:(n + 1) * 512], True, True, reads=['mgT', 'mb2'], writes=[('pY', n)])
                        S.copy('act', yacc[:, s_, n * 512:(n + 1) * 512], ps[:], reads=[('pY', n)], writes=[('y', s_, n), ('pY', n)])
                nxt = load_expert(0)
                S.barrier()
                for e in range(NEX):
                    W = nxt
                    S.sync_keys(W[4])
                    if e + 1 < NEX:
                        nxt = load_expert(e + 1)
                    if True:
                        if FORCE:
                            Q = S.unit_begin()
                            if FORCE == 'sparse':
                                sparse_unit(Q, e, W, nt)
                            else:
                                for (c0, N) in subs:
                                    dense_unit(Q, e, W, c0, N)
                            S.unit_end(Q)
                        else:
                            if not hasattr(self, 'flag_regs'):
                                self.flag_regs = nc.alloc_registers("moeflag", engines=mybir.ALL_ENGINES)
                            nc.regs_load(self.flag_regs, flagi[0:1, e:e + 1])
                            Q = S.unit_begin()
                            with nc.If_eq(self.flag_regs, 0):
                                sparse_unit(Q, e, W, nt)
                                S.unit_end(Q)
                            Q2 = Sched(nc, S.stack, parent=S, setid=Q.setid)
                            with nc.Else():
                                for (c0, N) in subs:
                                    dense_unit(Q2, e, W, c0, N)
                                S.unit_end(Q2)
                        S.unit_done()
                hs = S.sem(('HS',))
                for e_ in ENG:
                    S.E[e_].wait_ge(hs, S.units)
                for s_ in range(nt):
                    gi = t0 + s_
                    t = 1 if gi in ctx_tiles else 0
                    S.dma(hxv, h1d[gi * 128:(gi + 1) * 128, :], writes=['mhx'])
                    S.tt('dve', yacc[:, s_, :], yacc[:, s_, :], G2[:, t, :], ALU.mult, writes=[('yy', s_)])
                    S.tt('pool', hxv, yacc[:, s_, :], hxv, ALU.add, reads=[('yy', s_), 'mhx'], writes=['mhx'])
                    sink(gi, hxv, 'mhx', yacc[:, s_, :], ('yy', s_))
                S.barrier()
                t0 += nt
            S.barrier()

    def build(self):
        with self.top:
            self._build()
            self.S.finish()
        return self.nc

    def _build(self):
        S, nc = self.S, self.nc
        self.consts()
        G2 = self.sb(self.top, "G2", [128, 2, 1024], F32)
        self.h2 = self.scr("h2", [NQ, 1024], F32)
        with ExitStack() as L0:
            MT = [self.sb(L0, f"MT{t}", [128, 6144], F32) for t in range(2)]
            self.mod_phase(0, MT)
            if 'MT' in self.dbg:
                o = nc.dram_tensor("MTd", [2, 128, 6144], F32, kind="ExternalOutput").ap()
                for t in range(2):
                    for g in range(12):
                        S.dma(o[t][:, g * 512:(g + 1) * 512], MT[t][:, g * 512:(g + 1) * 512])
            if self.stop == 'mod':
                return
            self.ret_lg(L0)
            self.l0_A(MT)
            if self.stop == 'A':
                return
            self.l0_R()
            if self.stop == 'R':
                return
            self.l0_D()
            if self.stop == 'D':
                return
            self.l0_F(MT)
            for t in range(2):
                S.copy('dve', G2[:, t, :], MT[t][:, 5120:6144], writes=['G2'])
            S.barrier()
            if self.stop == 'F':
                return

        def sink0(gi, h2, h2k, scr=None, scrk=None):
            S.dma(self.h2[gi * 128:(gi + 1) * 128, :], h2, reads=[h2k], q='sp')
        self.moe2(0, G2, self.fT0, self.gT0, self.fTok0, self.gTok0, self.h1, NT_Q, 7, (0, 1), sink0)
        if self.stop == 'M0':
            return
        fnt = G2[:, 1:2, :].rearrange("p o d -> p (o d)")
        S.barrier()
        S.dma(fnt, self.fnw.partition_broadcast(128), writes=['fnt'])
        frs = Ring(nc, self.top, "frs", 3, [128, 4], F32)
        with ExitStack() as L1:
            MT = [self.sb(L1, f"MU{t}", [128, 6144], F32) for t in range(2)]
            self.mod_phase(1, MT)
            self.l1_A(MT)
            if self.stop == 'A1':
                return
            self.l1_W(MT)
            S.copy('dve', G2[:, 0, :], MT[0][:, 5120:6144], writes=['G2'])
            S.barrier()
            if self.stop == 'W1':
                return

        def sink1(gi, h2, h2k, scr=None, scrk=None):
            rs, rk = frs.next()
            S.act(scr, h2, AF.Square, reads=[h2k], writes=[scrk, rk], accum_out=rs[:, 0:1])
            S.ts('dve', rs[:, 1:2], rs[:, 0:1], 1.0 / 1024, 1e-6, ALU.mult, ALU.add, reads=[rk], writes=[rk])
            S.op('act', lambda E: E.sqrt(rs[:, 2:3], rs[:, 1:2]), reads=[rk], writes=[rk])
            S.op('dve', lambda E: E.reciprocal(rs[:, 3:4], rs[:, 2:3]), reads=[rk], writes=[rk])
            S.stt('dve', scr, h2, rs[:, 3:4], fnt, ALU.mult, ALU.mult, reads=[h2k, rk, 'fnt'], writes=[scrk])
            S.dma(self.out[gi * 128:(gi + 1) * 128, :], scr, reads=[scrk], q='sp')
        self.moe2(1, G2, self.fT1, self.gT1, self.fTok1, self.gTok1, self.h3, 32, 7, (), sink1)


def host_prep(inp, cid):
    b, half = cid // 2, cid % 2
    x, ctx = inp['x'][b], inp['ctx'][b]
    pos = np.arange(SEQ)
    if half == 1:
        x, ctx, pos = x[::-1], ctx[::-1], pos[::-1]
    m = {}
    m['xl'] = np.ascontiguousarray(np.concatenate([ctx, x], axis=0), dtype=np.float32)
    inv = (10000.0 ** (-np.arange(16, dtype=np.float32) / 16)).astype(np.float32)
    row = (pos // 64).astype(np.float32); col = (pos % 64).astype(np.float32)
    ar = row[:, None] * inv[None, :]; ac = col[:, None] * inv[None, :]
    rp = np.concatenate([np.cos(ar), np.cos(ac), np.sin(ar), np.sin(ac)], axis=1).astype(np.float32)
    rp0 = np.zeros((CTXL, 64), np.float32); rp0[:, 0:32] = 1.0
    m['rope'] = np.ascontiguousarray(np.concatenate([rp0, rp], axis=0))
    cc = np.stack([inp['c'][b], inp['c_ctx']], axis=-1)
    m['cc'] = np.ascontiguousarray(cc.reshape(8, 128, 2).transpose(1, 0, 2).reshape(128, 16))
    j = np.arange(128)[:, None].astype(np.float32); i = np.arange(128)[None, :].astype(np.float32)
    cst = np.zeros((128, 898), np.float32)
    cst[:, 0:128] = np.eye(128)
    cst[:, 128:256] = np.maximum(i - j, 0); cst[:, 256:384] = (i >= j)
    cst[:, 384:512] = np.maximum(j - i, 0); cst[:, 512:640] = (j >= i)
    cst[:, 640:768] = i + 1 + 0 * j; cst[:, 768:896] = 128 - i + 0 * j
    cst[:, 896] = 127 - j[:, 0]; cst[:, 897] = j[:, 0]
    m['cst'] = cst
    sel = np.zeros((32, 32, 128), np.float32)
    for e in range(32):
        sel[e, e, :] = 1.0
    m['sel'] = sel.reshape(32, 4096)
    for k in ('mod_w', 'mod_b', 'norm1_w', 'norm2_w', 'router_w', 'router_b', 'moe_w1', 'moe_w2', 'moe_b2'):
        m[k] = inp[k]
    m['final_norm_w'] = inp['final_norm_w'].reshape(1, 1024)
    m['even_w_in'] = inp['even_w_in'][0]; m['even_w_out'] = inp['even_w_out'][0]
    m['diff_lam'] = inp['diff_lam'].reshape(1, 256); m['diff_norm_w'] = inp['diff_norm_w'].reshape(128, 1)
    rdl = inp['ret_decay_logit'][0]
    if half == 1:
        rdl = rdl[::-1]
    m['ret_decay_logit'] = np.ascontiguousarray(rdl).reshape(1, 8)
    m['ret_norm_w'] = inp['ret_norm_w'].reshape(128, 1)
    m['odd_w_qkv'] = inp['odd_w_qkv'][0]; m['odd_w_out'] = inp['odd_w_out'][0]; m['odd_sinks'] = inp['odd_sinks'].reshape(1, 16)
    m['moe_b1'] = np.ascontiguousarray(inp['moe_b1'].reshape(2, 32, 16, 128).transpose(0, 1, 3, 2))
    return {k: np.ascontiguousarray(v, dtype=np.float32) for k, v in m.items()}


def kernel(**inputs):
    inp = {k: np.asarray(v) for k, v in inputs.items()}
    nc = K().build()
    in_maps = [host_prep(inp, cid) for cid in range(8)]
    res = run_bass_kernel_spmd(nc, in_maps, core_ids=list(range(8)))
    out = np.zeros((4, SEQ, 1024), np.float32)
    for cid in range(8):
        b, half = cid // 2, cid % 2
        o = res.results[cid]["out"]
        if half == 0:
            out[b, 0:4096] = o
        else:
            out[b, 4096:] = o[::-1]
    return out
```
